# Optimizing a Trainium2 kernel written in Bass

```python
import jax, jax.numpy as jnp
from jax import lax
import numpy as np

D_MODEL = 1024
BATCH = 8
SEQ = 2048
DEPTH = 1

D_MIX = D_MODEL
D_LRU = D_MIX // 2
D_CONV = D_MIX - D_LRU
LRU_HEADS = 8
LRU_HEAD_DIM = D_LRU // LRU_HEADS
LRU_CONV_WIDTH = 4
LRU_C = 8.0
LRU_A_MIN = 0.9
LRU_A_MAX = 0.999
CONV_GROUPS = 8
CONV_GROUP_DIM = D_CONV // CONV_GROUPS
CONF_KERNEL = 31
PEER_HEADS = 8
PEER_N_KEYS = 128
PEER_N_EXPERTS = PEER_N_KEYS ** 2
PEER_D_QUERY = 256
PEER_HALF = PEER_D_QUERY // 2
PEER_TOPK = 16
PEER_BLOCK = 128
EPS = 1e-6

kernel_name = "hybrid_rglru_conformer_peer_encoder"


def rms_norm(x, g):
    xf = x.astype(jnp.float32)
    y = xf * lax.rsqrt(jnp.mean(xf * xf, axis=-1, keepdims=True) + EPS)
    return (y * g.astype(jnp.float32)).astype(x.dtype)


def group_layer_norm(x, g, b):
    B_, S_, C = x.shape
    xg = x.astype(jnp.float32).reshape(B_, S_, CONV_GROUPS, CONV_GROUP_DIM)
    mu = jnp.mean(xg, axis=-1, keepdims=True)
    var = jnp.mean(jnp.square(xg - mu), axis=-1, keepdims=True)
    y = ((xg - mu) * lax.rsqrt(var + EPS)).reshape(B_, S_, C)
    return (y * g.astype(jnp.float32) + b.astype(jnp.float32)).astype(x.dtype)


def depthwise_conv(x, w, b, pad):
    C = x.shape[-1]
    y = lax.conv_general_dilated(
        x, w[:, None, :].astype(x.dtype), window_strides=(1,), padding=[pad],
        dimension_numbers=("NWC", "WIO", "NWC"), feature_group_count=C)
    return y + b.astype(x.dtype)


def block_diag_linear(x, w, b):
    B_, S_, _ = x.shape
    xh = x.reshape(B_, S_, LRU_HEADS, LRU_HEAD_DIM)
    y = jnp.einsum("bshi,hij->bshj", xh, w.astype(jnp.float32))
    return y.reshape(B_, S_, D_LRU) + b.astype(jnp.float32)


def _linear_recurrence(left, right):
    a_l, b_l = left
    a_r, b_r = right
    return a_l * a_r, a_r * b_l + b_r


def rg_lru(x, w_r, b_r, w_i, b_i, lam, reverse):
    xf = x.astype(jnp.float32)
    r = jax.nn.sigmoid(block_diag_linear(xf, w_r, b_r))
    i = jax.nn.sigmoid(block_diag_linear(xf, w_i, b_i))
    log_a = LRU_C * r * jax.nn.log_sigmoid(lam.astype(jnp.float32))
    a = jnp.exp(log_a)
    u = jnp.sqrt(-jnp.expm1(2.0 * log_a)) * (i * xf)
    _, h = lax.associative_scan(_linear_recurrence, (a, u), reverse=reverse, axis=1)
    return h


def hybrid_mixer(n, w_in, lru_conv_w, lru_conv_b, lru_w_rg, lru_b_rg, lru_w_ig, lru_b_ig,
                 lru_lambda, conf_conv_w, conf_conv_b, conf_norm_g, conf_norm_b,
                 beta_lru, beta_conv, w_out):
    z = jnp.einsum("bsd,de->bse", n, w_in.astype(n.dtype))
    x_lru, g_lru, a_glu, b_glu = jnp.split(
        z, [D_LRU, 2 * D_LRU, 2 * D_LRU + D_CONV], axis=-1)

    lpad = LRU_CONV_WIDTH // 2
    xc = depthwise_conv(x_lru, lru_conv_w, lru_conv_b, (lpad, LRU_CONV_WIDTH - 1 - lpad))
    h_fwd = rg_lru(xc, lru_w_rg[0], lru_b_rg[0], lru_w_ig[0], lru_b_ig[0], lru_lambda[0], False)
    h_bwd = rg_lru(xc, lru_w_rg[1], lru_b_rg[1], lru_w_ig[1], lru_b_ig[1], lru_lambda[1], True)
    y_lru = ((h_fwd + h_bwd) * jax.nn.gelu(g_lru.astype(jnp.float32))).astype(n.dtype)

    glu = a_glu * jax.nn.sigmoid(b_glu)
    c = depthwise_conv(glu, conf_conv_w, conf_conv_b, (CONF_KERNEL // 2, CONF_KERNEL // 2))
    y_conv = jax.nn.silu(group_layer_norm(c, conf_norm_g, conf_norm_b))

    y = jnp.concatenate([rms_norm(y_lru, beta_lru), rms_norm(y_conv, beta_conv)], axis=-1)
    return jnp.einsum("bse,ed->bsd", y, w_out.astype(y.dtype))


def peer_ffn(n, w_q, sub_keys, expert_u, expert_v):
    B_, S_, D = n.shape
    n_blocks = (B_ * S_) // PEER_BLOCK
    xb = n.reshape(n_blocks, PEER_BLOCK, D)

    def retrieve(xt):
        q = jnp.einsum("td,de->te", xt, w_q.astype(xt.dtype))
        q = q.reshape(PEER_BLOCK, PEER_HEADS, 2, PEER_HALF).astype(jnp.float32)
        s = jnp.einsum("thpc,hpkc->thpk", q, sub_keys.astype(jnp.float32))
        s1, i1 = lax.top_k(s[:, :, 0], PEER_TOPK)
        s2, i2 = lax.top_k(s[:, :, 1], PEER_TOPK)
        cand_s = (s1[..., :, None] + s2[..., None, :]).reshape(PEER_BLOCK, PEER_HEADS, PEER_TOPK * PEER_TOPK)
        cand_i = (i1[..., :, None] * PEER_N_KEYS + i2[..., None, :]).reshape(PEER_BLOCK, PEER_HEADS, PEER_TOPK * PEER_TOPK)
        top_s, pos = lax.top_k(cand_s, PEER_TOPK)
        idx = jnp.take_along_axis(cand_i, pos, axis=-1)
        gate = jax.nn.softmax(top_s, axis=-1)
        u = expert_u[idx]
        v = expert_v[idx]
        act = jax.nn.gelu(jnp.einsum("thkd,td->thk", u, xt))
        return jnp.einsum("thk,thkd->td", (gate * act).astype(v.dtype), v)

    out = lax.map(retrieve, xb)
    return out.reshape(B_, S_, D).astype(n.dtype)


def setup_inputs(seed: int = 0) -> dict:
    key = jax.random.key(seed)
    ks = jax.random.split(key, 24)
    L = DEPTH
    nrm = lambda k, shape, scale: jax.random.normal(k, shape, jnp.float32) * scale
    gain = lambda k, shape: 1.0 + 0.02 * jax.random.normal(k, shape, jnp.float32)
    a0 = jax.random.uniform(ks[9], (L, 2, D_LRU), jnp.float32, LRU_A_MIN, LRU_A_MAX)
    p = a0 ** (1.0 / LRU_C)
    lru_lambda = jnp.log(p) - jnp.log1p(-p)
    return {
        "x": nrm(ks[0], (BATCH, SEQ, D_MODEL), 1.0),
        "mix_norm_g": gain(ks[1], (L, D_MODEL)),
        "w_in": nrm(ks[2], (L, D_MODEL, 2 * D_LRU + 2 * D_CONV), D_MODEL ** -0.5),
        "lru_conv_w": nrm(ks[3], (L, LRU_CONV_WIDTH, D_LRU), LRU_CONV_WIDTH ** -0.5),
        "lru_conv_b": nrm(ks[4], (L, D_LRU), 0.02),
        "lru_w_rg": nrm(ks[5], (L, 2, LRU_HEADS, LRU_HEAD_DIM, LRU_HEAD_DIM), LRU_HEAD_DIM ** -0.5),
        "lru_b_rg": nrm(ks[6], (L, 2, D_LRU), 0.02),
        "lru_w_ig": nrm(ks[7], (L, 2, LRU_HEADS, LRU_HEAD_DIM, LRU_HEAD_DIM), LRU_HEAD_DIM ** -0.5),
        "lru_b_ig": nrm(ks[8], (L, 2, D_LRU), 0.02),
        "lru_lambda": lru_lambda,
        "conf_conv_w": nrm(ks[10], (L, CONF_KERNEL, D_CONV), CONF_KERNEL ** -0.5),
        "conf_conv_b": nrm(ks[11], (L, D_CONV), 0.02),
        "conf_norm_g": gain(ks[12], (L, D_CONV)),
        "conf_norm_b": nrm(ks[13], (L, D_CONV), 0.02),
        "beta_lru": gain(ks[14], (L, D_LRU)),
        "beta_conv": gain(ks[15], (L, D_CONV)),
        "w_out": nrm(ks[16], (L, D_MIX, D_MODEL), D_MIX ** -0.5),
        "ffn_norm_g": gain(ks[17], (L, D_MODEL)),
        "peer_w_q": nrm(ks[18], (L, D_MODEL, PEER_HEADS * PEER_D_QUERY), D_MODEL ** -0.5),
        "peer_sub_keys": nrm(ks[19], (L, PEER_HEADS, 2, PEER_N_KEYS, PEER_HALF), PEER_HALF ** -0.5),
        "peer_u": nrm(ks[20], (L, PEER_N_EXPERTS, D_MODEL), D_MODEL ** -0.5),
        "peer_v": nrm(ks[21], (L, PEER_N_EXPERTS, D_MODEL), PEER_HEADS ** -0.5),
        "final_norm_g": gain(ks[22], (D_MODEL,)),
    }


def reference(x, mix_norm_g, w_in, lru_conv_w, lru_conv_b, lru_w_rg, lru_b_rg, lru_w_ig,
              lru_b_ig, lru_lambda, conf_conv_w, conf_conv_b, conf_norm_g, conf_norm_b,
              beta_lru, beta_conv, w_out, ffn_norm_g, peer_w_q, peer_sub_keys, peer_u,
              peer_v, final_norm_g):
    h = x
    for l in range(DEPTH):
        h = h + hybrid_mixer(
            rms_norm(h, mix_norm_g[l]), w_in[l], lru_conv_w[l], lru_conv_b[l],
            lru_w_rg[l], lru_b_rg[l], lru_w_ig[l], lru_b_ig[l], lru_lambda[l],
            conf_conv_w[l], conf_conv_b[l], conf_norm_g[l], conf_norm_b[l],
            beta_lru[l], beta_conv[l], w_out[l])
        h = h + peer_ffn(rms_norm(h, ffn_norm_g[l]), peer_w_q[l], peer_sub_keys[l],
                         peer_u[l], peer_v[l])
    return rms_norm(h, final_norm_g)
```

```python
import numpy as np
from contextlib import ExitStack
import concourse.bass as bass
import concourse.mybir as mybir
from concourse.bass_utils import run_bass_kernel_spmd

F32 = mybir.dt.float32
BF16 = mybir.dt.bfloat16
U32 = mybir.dt.uint32
I32 = mybir.dt.int32
AF = mybir.ActivationFunctionType
ALU = mybir.AluOpType
AX = mybir.AxisListType

T = 2048
D = 1024
EPS = 1e-6
NPP = 196
WKC = 2056


class Prog:
    def __init__(self, nc):
        self.nc = nc
        self.ops = {e: [] for e in ("sync", "scalar", "vector", "gpsimd", "tensor")}
        self.cnt = {}
        self.waited = {e: {} for e in self.ops}
        self.last_w = {}
        self.readers = {}
        self.semkeys = []

    def _semkey(self, k):
        if k not in self.cnt:
            self.cnt[k] = 0
            self.semkeys.append(k)
        return k

    def _deps(self, eng, reads, writes, own_key, skip_same=False):
        need = {}

        def add(k, v):
            if skip_same and k == own_key:
                return
            if need.get(k, 0) < v:
                need[k] = v

        for b in reads:
            d = self.last_w.get(b)
            if d is not None:
                add(*d)
        for b in writes:
            d = self.last_w.get(b)
            if d is not None:
                add(*d)
            for k, v in self.readers.get(b, {}).items():
                add(k, v)
        waits = []
        for k, v in need.items():
            if self.waited[eng].get(k, 0) < v:
                self.waited[eng][k] = v
                waits.append((k, v))
        return waits

    def _commit(self, reads, writes, key, val):
        for b in reads:
            r = self.readers.setdefault(b, {})
            if r.get(key, 0) < val:
                r[key] = val
        for b in writes:
            self.last_w[b] = (key, val)
            self.readers[b] = {}

    @staticmethod
    def _ispsum(k):
        return k == "QP" or (isinstance(k, tuple) and k[0] in ("pb", "SP", "xbp", "op"))

    def op(self, eng, fn, reads=(), writes=()):
        ps = [r for r in reads if self._ispsum(r)]
        if ps:
            reads = [r for r in reads if not self._ispsum(r)]
            writes = list(writes) + ps
        key = self._semkey(eng)
        waits = self._deps(eng, reads, writes, key, skip_same=(eng == "tensor"))
        self.cnt[key] += 1
        self.ops[eng].append((waits, fn, key, 1))
        self._commit(reads, writes, key, self.cnt[key])

    def dma(self, queue, fn, reads=(), writes=(), stream=None):
        if stream is None:
            stream = tuple(writes)
        key = self._semkey(("dma", stream))
        waits = self._deps(queue, reads, writes, key)
        self.cnt[key] += 16
        self.ops[queue].append((waits, fn, key, 16))
        self._commit(reads, writes, key, self.cnt[key])

    def barrier(self):
        for e in self.ops:
            waits = []
            for k in self.semkeys:
                v = self.cnt[k]
                if v > 0 and self.waited[e].get(k, 0) < v:
                    self.waited[e][k] = v
                    waits.append((k, v))
            self.ops[e].append((waits, None, None, 0))
        self.last_w = {}
        self.readers = {}

    def finish(self, eng, bufs):
        waits = self._deps(eng, bufs, (), None)
        self.ops[eng].append((waits, None, None, 0))

    def emit(self):
        nc = self.nc
        with ExitStack() as st:
            sems = {}
            for i, k in enumerate(self.semkeys):
                sems[k] = st.enter_context(nc.semaphore("s%d" % i))
            block = st.enter_context(nc.Block())

            def run(engname):
                def body(e):
                    for waits, fn, key, inc in self.ops[engname]:
                        for k, v in waits:
                            e.wait_ge(sems[k], v)
                        if fn is not None:
                            fn(e).then_inc(sems[key], inc)
                return body

            block.sync(run("sync"))
            block.scalar(run("scalar"))
            block.vector(run("vector"))
            block.gpsimd(run("gpsimd"))
            block.tensor(run("tensor"))


def sub(ap, off, dims):
    return bass.AP(ap.tensor, ap.offset + off, [list(ap.ap[0])] + [list(d) for d in dims])


def build(stop_after=None, lvl=99):
    nc = bass.Bass("TRN2", target_bir_lowering=False)

    def dram(name, shape, dt=F32, kind="ExternalInput"):
        return nc.dram_tensor(name, shape, dt, kind=kind).ap()

    xT_d = dram("xT", [D, T])
    x_d = dram("x", [T, D])
    win_d = dram("w_in", [D, 2048])
    wout_d = dram("w_out", [D, D])
    wq_d = dram("w_q", [D, 2048])
    skT_d = dram("skT", [16, 128, 128])
    pu_d = dram("pu", [16384, D])
    pv_d = dram("pv", [16384, D])
    pp_d = dram("pp", [128, NPP])
    wg_d = dram("wg", [4, 8, 64, 64])
    g2_d = dram("g2", [1, D])
    gf_d = dram("gf", [1, D])
    out_d = dram("out", [T, D], kind="ExternalOutput")
    puv_d = dram("puv", [16384, 2 * D], BF16, kind="Internal")

    with ExitStack() as st:
        def sb(name, shape, dt):
            return st.enter_context(nc.sbuf_tensor(name, shape, dt))

        WK = [sb("wk%d" % i, [128, WKC], F32) for i in range(10)]
        XTB = sb("xtb", [128, 8 * T], BF16)
        WINB = sb("winb", [128, 8 * 2048], BF16)
        YB = sb("yb", [128, 8 * T], BF16)
        RSTD = sb("rstd", [128, T], F32)
        PP = sb("pp_sb", [128, NPP], F32)
        BDB = sb("bdb", [128, 2048], BF16)
        ident = sb("ident", [128, 128], F32)
        identb = sb("identb", [128, 128], BF16)
        ones = sb("ones", [128, 128], F32)
        GM = sb("gm", [128, 128], F32)
        SM = sb("sm", [128, 256], F32)
        CVI = sb("cvi", [128, 2048], F32)
        CVO = sb("cvo", [128, 2048], BF16)
        PS2 = [st.enter_context(nc.psum_tensor("ps%d" % i, [128, 1024], F32)) for i in range(4)]

        WKf = [w[:] for w in WK]
        WKb = [w[:].bitcast(BF16) for w in WK]
        XTBf = XTB[:].bitcast(F32)
        WINBf = WINB[:].bitcast(F32)
        YBf = YB[:].bitcast(F32)
        YBu = YB[:].bitcast(U32)
        RSTDb = RSTD[:].bitcast(BF16)

        def bank(b):
            return PS2[b // 2][:, (b % 2) * 512:(b % 2) * 512 + 512]

        def bk(b):
            return ("pb", b)

        CL = SM[:, 0:8]
        CL2 = SM[:, 8:16]
        EPSC = SM[:, 16:17]
        ONEC = SM[:, 17:18]
        LSG = SM[:, 18:26]
        RS2 = SM[:, 32:64]
        SS2 = SM[:, 64:80]
        RSB = SM[:, 80:96]
        SS3 = SM[:, 96:97]
        R3 = SM[:, 97:98]
        IOTA = SM[:, 128:144]
        IOTAi = SM[:, 144:160].bitcast(I32)

        p = Prog(nc)
        V, A, G, P_ = "vector", "scalar", "gpsimd", "tensor"

        cv_src = [pu_d.rearrange("(p r) d -> p (r d)", p=128), pv_d.rearrange("(p r) d -> p (r d)", p=128)]
        puv_v = puv_d.rearrange("(p r) d -> p r d", p=128)
        NPIECE = 256
        cvn = [0]

        def cv_in(i):
            if i >= NPIECE:
                return
            tb, j, sl = i // 128, i % 128, i % 2
            p.dma(G, (lambda e, tb=tb, j=j, sl=sl: e.dma_start(out=CVI[:, sl * 1024:(sl + 1) * 1024],
                                                             in_=cv_src[tb][:, j * 1024:(j + 1) * 1024])),
                  writes=[("cvi", sl)], stream=("cvi", sl))

        def cv(n):
            for _ in range(n):
                i = cvn[0]
                if i >= NPIECE:
                    return
                if i == 0:
                    cv_in(0)
                    cv_in(1)
                tb, j, sl = i // 128, i % 128, i % 2
                p.op(G, (lambda e, sl=sl: e.tensor_copy(CVO[:, sl * 1024:(sl + 1) * 1024], CVI[:, sl * 1024:(sl + 1) * 1024])),
                     reads=[("cvi", sl)], writes=[("cvo", sl)])
                p.dma(G, (lambda e, tb=tb, j=j, sl=sl: e.dma_start(out=puv_v[:, j, tb * 1024:(tb + 1) * 1024],
                                                                 in_=CVO[:, sl * 1024:(sl + 1) * 1024])),
                      reads=[("cvo", sl)], writes=[("cvd", i)], stream=("cvo", sl))
                cv_in(i + 2)
                cvn[0] += 1

        p.dma("sync", lambda e: e.dma_start(out=PP[:], in_=pp_d), writes=["PP"])
        p.op(G, lambda e: e.memset(ident[:], 0.0), writes=["ident"])
        p.op(G, lambda e: e.affine_select(out=ident[:], in_=ident[:], pattern=[[-1, 128]],
                                          compare_op=ALU.not_equal, fill=1.0, base=0,
                                          channel_multiplier=1), reads=["ident"], writes=["ident"])
        p.op(V, lambda e: e.tensor_copy(identb[:], ident[:]), reads=["ident"], writes=["identb"])
        p.op(G, lambda e: e.memset(ones[:], 1.0), writes=["ones"])
        p.op(G, lambda e: e.memset(GM[:], 0.0), writes=["GM"])
        p.op(G, lambda e: e.memset(GM[0:64, 0:64], 1.0 / 64), reads=["GM"], writes=["GM"])
        p.op(G, lambda e: e.memset(GM[64:128, 64:128], 1.0 / 64), reads=["GM"], writes=["GM"])
        p.op(G, lambda e: e.memset(SM[:], 0.0), writes=["SM"])
        p.op(G, lambda e: e.memset(EPSC, EPS), reads=["SM"], writes=["SM"])
        p.op(G, lambda e: e.memset(ONEC, 1.0), reads=["SM"], writes=["SM"])
        p.op(G, lambda e: e.iota(IOTAi, pattern=[[1, 16]], base=0, channel_multiplier=0),
             reads=["SM"], writes=["SM"])
        p.op(V, lambda e: e.tensor_copy(IOTA, IOTAi), reads=["SM"], writes=["SM"])
        p.op(G, lambda e: e.memset(WKf[0], 0.0), writes=["WK0"])
        p.op(G, lambda e: e.memset(WKf[3], 0.0), writes=["WK3"])
        p.op(G, lambda e: e.memset(WKf[9], 0.0), writes=["WK9"])
        for kind in range(4):
            for c in range(4):
                for hl in range(2):
                    col = (kind * 4 + c) * 128 + hl * 64
                    p.dma("sync", (lambda e, kind=kind, c=c, hl=hl, col=col: e.dma_start(
                        out=WK[9][hl * 64:(hl + 1) * 64, col:col + 64], in_=wg_d[kind, 2 * c + hl])),
                        writes=["WK9"])
        p.op(A, lambda e: e.copy(out=BDB[:], in_=WK[9][:, 0:2048]), reads=["WK9"], writes=["BDB"])
        p.op(A, lambda e: e.activation(out=LSG, in_=PP[:, 44:52], func=AF.Sigmoid), reads=["PP", "SM"], writes=["SM"])
        p.op(A, lambda e: e.activation(out=LSG, in_=LSG, func=AF.Ln), reads=["SM"], writes=["SM"])
        p.op(V, lambda e: e.tensor_scalar(CL, LSG, 8.0, None, ALU.mult), reads=["SM"], writes=["SM"])
        p.op(V, lambda e: e.tensor_scalar(CL2, LSG, 16.0, None, ALU.mult), reads=["SM"], writes=["SM"])

        for c in range(8):
            stg = 1 + c % 2
            sq = 4 + c % 2
            p.dma("sync", (lambda e, c=c, stg=stg: e.dma_start(out=WK[stg][:, 0:T], in_=xT_d[c * 128:(c + 1) * 128, :])),
                  writes=["WK%d" % stg])
            p.op(A, (lambda e, stg=stg, sq=sq: e.activation(out=WK[sq][:, 0:T], in_=WK[stg][:, 0:T], func=AF.Square)),
                 reads=["WK%d" % stg], writes=["WK%d" % sq])
            p.op(V, (lambda e, c=c, stg=stg: e.tensor_copy(XTB[:, c * T:(c + 1) * T], WK[stg][:, 0:T])),
                 reads=["WK%d" % stg], writes=["XTB"])
            for tt in range(4):
                p.op(P_, (lambda e, c=c, sq=sq, tt=tt: e.matmul(bank(tt), ones[:], WK[sq][:, tt * 512:(tt + 1) * 512],
                                                                 start=(c == 0), stop=(c == 7))),
                     reads=["ones", "WK%d" % sq], writes=[bk(tt)])
        cv(16)
        for tt in range(4):
            p.op(A, (lambda e, tt=tt: e.activation(out=RSTD[:, tt * 512:(tt + 1) * 512], in_=bank(tt), func=AF.Sqrt,
                                                   bias=EPSC, scale=1.0 / D)),
                 reads=[bk(tt), "SM"], writes=["RSTD"])
        p.op(V, lambda e: e.reciprocal(RSTD[:], RSTD[:]), reads=["RSTD"], writes=["RSTD"])
        for c in range(8):
            stg = 6 + c % 2
            p.dma("sync", (lambda e, c=c, stg=stg: e.dma_start(out=WK[stg][:, 0:2048], in_=win_d[c * 128:(c + 1) * 128, :])),
                  writes=["WK%d" % stg])
            p.op(V, (lambda e, c=c, stg=stg: e.tensor_scalar(WINB[:, c * 2048:(c + 1) * 2048], WK[stg][:, 0:2048],
                                                             PP[:, c:c + 1], 0.0, ALU.mult, ALU.add)),
                 reads=["WK%d" % stg, "PP"], writes=["WINB"])

        zcnt = [0]

        def zchunk(f, dst, dkey, doff):
            for tt in range(4):
                b = 4 + zcnt[0] % 2
                zcnt[0] += 1
                for dc in range(8):
                    p.op(P_, (lambda e, b=b, dc=dc, tt=tt, f=f: e.matmul(
                        bank(b), WINB[:, dc * 2048 + f * 128: dc * 2048 + f * 128 + 128],
                        XTB[:, dc * T + tt * 512: dc * T + tt * 512 + 512], start=(dc == 0), stop=(dc == 7))),
                        reads=["WINB", "XTB"], writes=[bk(b)])
                p.op(V, (lambda e, b=b, tt=tt, dst=dst, doff=doff: e.tensor_tensor(
                    dst[:, doff + tt * 512: doff + tt * 512 + 512], bank(b), RSTD[:, tt * 512:(tt + 1) * 512], ALU.mult)),
                    reads=[bk(b), "RSTD"], writes=[dkey])

        gcnt = [0]

        def gbank():
            b = 6 + gcnt[0] % 2
            gcnt[0] += 1
            return b

        def sumsq_cols(src, skey, colbase):
            for ti in range(16):
                p.op(P_, (lambda e, ti=ti, src=src, colbase=colbase: e.matmul(
                    bank(0)[:, colbase + ti: colbase + ti + 1], src[:, ti * 128:(ti + 1) * 128], ones[:, 0:1],
                    start=True, stop=True)), reads=[skey, "ones"], writes=[bk(0)])

        for c in range(4):
            XL, XC, TMP, XCBt = WKf[0], WKf[1], WKf[6], WKb[7]
            zchunk(c, XL, "WK0", 2)
            p.op(V, (lambda e, c=c: e.tensor_scalar(XC[:, 0:T], XL[:, 0:T], PP[:, 8 + c:9 + c], PP[:, 24 + c:25 + c],
                                                    ALU.mult, ALU.add)), reads=["WK0", "PP"], writes=["WK1"])
            for k in range(1, 4):
                p.op(V, (lambda e, c=c, k=k: e.scalar_tensor_tensor(
                    out=XC[:, 0:T], in0=XL[:, k:k + T], scalar=PP[:, 8 + k * 4 + c: 9 + k * 4 + c], in1=XC[:, 0:T],
                    op0=ALU.mult, op1=ALU.add)), reads=["WK0", "WK1", "PP"], writes=["WK1"])
            p.op(A, lambda e: e.copy(out=XCBt[:, 0:T], in_=XC[:, 0:T]), reads=["WK1"], writes=["WK7"])
            for kind in range(4):
                Gt = WKf[2 + kind]
                for tt in range(4):
                    b = gbank()
                    p.op(P_, (lambda e, b=b, kind=kind, c=c, tt=tt: e.matmul(
                        bank(b), BDB[:, (kind * 4 + c) * 128:(kind * 4 + c) * 128 + 128],
                        XCBt[:, tt * 512:(tt + 1) * 512], start=True, stop=True)),
                        reads=["BDB", "WK7"], writes=[bk(b)])
                    p.op(A, (lambda e, b=b, kind=kind, c=c, tt=tt, Gt=Gt: e.activation(
                        out=Gt[:, tt * 512:(tt + 1) * 512], in_=bank(b), func=AF.Sigmoid,
                        bias=PP[:, 28 + kind * 4 + c: 29 + kind * 4 + c])),
                        reads=[bk(b), "PP"], writes=["WK%d" % (2 + kind)])
            for d_ in range(2):
                R, I_, H = WKf[2 + 2 * d_], WKf[3 + 2 * d_], WKf[8 + d_]
                rk, ik, hk = "WK%d" % (2 + 2 * d_), "WK%d" % (3 + 2 * d_), "WK%d" % (8 + d_)
                col = d_ * 4 + c
                p.op(A, (lambda e, R=R, col=col: e.activation(out=TMP[:, 0:T], in_=R[:, 0:T], func=AF.Exp,
                                                               scale=CL2[:, col:col + 1])),
                     reads=[rk, "SM"], writes=["WK6"])
                p.op(A, (lambda e: e.activation(out=TMP[:, 0:T], in_=TMP[:, 0:T], func=AF.Sqrt, bias=ONEC, scale=-1.0)),
                     reads=["WK6", "SM"], writes=["WK6"])
                p.op(A, (lambda e, R=R, col=col: e.activation(out=R[:, 0:T], in_=R[:, 0:T], func=AF.Exp,
                                                               scale=CL[:, col:col + 1])),
                     reads=[rk, "SM"], writes=[rk])
                p.op(V, (lambda e, I_=I_: e.tensor_tensor(I_[:, 0:T], I_[:, 0:T], XC[:, 0:T], ALU.mult)),
                     reads=[ik, "WK1"], writes=[ik])
                p.op(V, (lambda e, I_=I_: e.tensor_tensor(I_[:, 0:T], I_[:, 0:T], TMP[:, 0:T], ALU.mult)),
                     reads=[ik, "WK6"], writes=[ik])
                if d_ == 0:
                    p.op(V, (lambda e, R=R, I_=I_, H=H: e.tensor_tensor_scan(
                        out=H[:, 0:T], data0=R[:, 0:T], data1=I_[:, 0:T], initial=0.0, op0=ALU.mult, op1=ALU.add)),
                        reads=[rk, ik], writes=[hk])
                else:
                    p.op(V, (lambda e, R=R, I_=I_, H=H: e.tensor_tensor_scan(
                        out=H[:, 0:T][:, ::-1], data0=R[:, 0:T][:, ::-1],
                        data1=I_[:, 0:T][:, ::-1], initial=0.0, op0=ALU.mult, op1=ALU.add)),
                        reads=[rk, ik], writes=[hk])
            GL = WKf[7]
            zchunk(4 + c, GL, "WK7", 0)
            p.op(A, lambda e: e.activation(out=GL[:, 0:T], in_=GL[:, 0:T], func=AF.Gelu_apprx_tanh),
                 reads=["WK7"], writes=["WK7"])
            H0, H1 = WKf[8], WKf[9]
            p.op(G, lambda e: e.tensor_tensor(H0[:, 0:T], H0[:, 0:T], H1[:, 0:T], ALU.add), reads=["WK8", "WK9"], writes=["WK8"])
            p.op(V, lambda e: e.tensor_tensor(H0[:, 0:T], H0[:, 0:T], GL[:, 0:T], ALU.mult), reads=["WK8", "WK7"], writes=["WK8"])
            p.op(A, (lambda e, c=c: e.copy(out=YB[:, c * T:(c + 1) * T], in_=H0[:, 0:T])), reads=["WK8"], writes=["YB"])
            p.op(G, lambda e: e.tensor_tensor(H1[:, 0:T], H0[:, 0:T], H0[:, 0:T], ALU.mult), reads=["WK8"], writes=["WK9"])
            sumsq_cols(H1, "WK9", c * 16)
            cv(12)

        p.op(G, lambda e: e.memset(WKb[3][:, 0:16], 0.0), writes=["WK3"])
        p.op(G, lambda e: e.memset(WKb[3][:, 14 + T:32 + T], 0.0), writes=["WK3"])
        for c in range(4):
            Aa, Bb, GLU, DG, CV, Dd, DSQ, RS = WKf[1], WKf[2], WKb[3], WKb[4], WKf[5], WKf[6], WKf[7], WKf[8]
            zchunk(8 + c, Aa, "WK1", 0)
            zchunk(12 + c, Bb, "WK2", 0)
            p.op(A, lambda e: e.activation(out=Bb[:, 0:T], in_=Bb[:, 0:T], func=AF.Sigmoid), reads=["WK2"], writes=["WK2"])
            p.op(V, lambda e: e.tensor_tensor(GLU[:, 15:15 + T], Aa[:, 0:T], Bb[:, 0:T], ALU.mult),
                 reads=["WK1", "WK2"], writes=["WK3"])
            for k in range(31):
                p.op(V, (lambda e, k=k, c=c: e.tensor_scalar(DG[:, k * 128:(k + 1) * 128], identb[:],
                                                             PP[:, 52 + k * 4 + c: 53 + k * 4 + c], 0.0, ALU.mult, ALU.add)),
                     reads=["identb", "PP"], writes=["WK4"])
            for tt in range(4):
                b = gbank()
                for k in range(31):
                    p.op(P_, (lambda e, b=b, k=k, tt=tt: e.matmul(bank(b), DG[:, k * 128:(k + 1) * 128],
                                                                  GLU[:, tt * 512 + k: tt * 512 + k + 512],
                                                                  start=(k == 0), stop=(k == 30))),
                         reads=["WK4", "WK3"], writes=[bk(b)])
                p.op(A, (lambda e, b=b, tt=tt, c=c: e.activation(out=CV[:, tt * 512:(tt + 1) * 512], in_=bank(b),
                                                                 func=AF.Identity, bias=PP[:, 176 + c:177 + c])),
                     reads=[bk(b), "PP"], writes=["WK5"])
            for tt in range(4):
                b = gbank()
                p.op(P_, (lambda e, b=b, tt=tt: e.matmul(bank(b), GM[:], CV[:, tt * 512:(tt + 1) * 512], start=True, stop=True)),
                     reads=["GM", "WK5"], writes=[bk(b)])
                p.op(V, (lambda e, b=b, tt=tt: e.tensor_tensor(Dd[:, tt * 512:(tt + 1) * 512], CV[:, tt * 512:(tt + 1) * 512],
                                                               bank(b), ALU.subtract)),
                     reads=[bk(b), "WK5"], writes=["WK6"])
            p.op(G, lambda e: e.tensor_tensor(DSQ[:, 0:T], Dd[:, 0:T], Dd[:, 0:T], ALU.mult), reads=["WK6"], writes=["WK7"])
            for tt in range(4):
                b = gbank()
                p.op(P_, (lambda e, b=b, tt=tt: e.matmul(bank(b), GM[:], DSQ[:, tt * 512:(tt + 1) * 512], start=True, stop=True)),
                     reads=["GM", "WK7"], writes=[bk(b)])
                p.op(A, (lambda e, b=b, tt=tt: e.activation(out=RS[:, tt * 512:(tt + 1) * 512], in_=bank(b), func=AF.Sqrt,
                                                            bias=EPSC, scale=1.0)),
                     reads=[bk(b), "SM"], writes=["WK8"])
            p.op(V, lambda e: e.reciprocal(RS[:, 0:T], RS[:, 0:T]), reads=["WK8"], writes=["WK8"])
            p.op(V, lambda e: e.tensor_tensor(Dd[:, 0:T], Dd[:, 0:T], RS[:, 0:T], ALU.mult), reads=["WK6", "WK8"], writes=["WK6"])
            p.op(A, (lambda e, c=c: e.activation(out=Dd[:, 0:T], in_=Dd[:, 0:T], func=AF.Silu,
                                                 bias=PP[:, 184 + c:185 + c], scale=PP[:, 180 + c:181 + c])),
                 reads=["WK6", "PP"], writes=["WK6"])
            p.op(A, (lambda e, c=c: e.copy(out=YB[:, (4 + c) * T:(5 + c) * T], in_=Dd[:, 0:T])), reads=["WK6"], writes=["YB"])
            p.op(G, lambda e: e.tensor_tensor(DSQ[:, 0:T], Dd[:, 0:T], Dd[:, 0:T], ALU.mult), reads=["WK6"], writes=["WK7"])
            sumsq_cols(DSQ, "WK7", 64 + c * 16)
            cv(12)

        p.barrier()
        p.op(V, lambda e: e.tensor_reduce(out=sub(RS2, 0, [[16, 2], [1, 16]]), in_=sub(bank(0), 0, [[64, 2], [1, 16], [16, 4]]), axis=AX.X, op=ALU.add),
             reads=[bk(0)], writes=["SMr"])
        p.op(A, lambda e: e.activation(out=RS2, in_=RS2, func=AF.Sqrt, bias=EPSC, scale=1.0 / 512), reads=["SMr", "SM"], writes=["SMr"])
        p.op(V, lambda e: e.reciprocal(RS2, RS2), reads=["SMr"], writes=["SMr"])
        for cc in range(8):
            stg = WK[2][:, (cc % 2) * 1024:(cc % 2) * 1024 + 1024]
            skey = "WK2_%d" % (cc % 2)
            wob = WKb[cc // 4][:, (cc % 4) * 1024:(cc % 4) * 1024 + 1024]
            p.dma("sync", (lambda e, cc=cc, stg=stg: e.dma_start(out=stg, in_=wout_d[cc * 128:(cc + 1) * 128, :])),
                  writes=[skey])
            p.op(V, (lambda e, cc=cc, stg=stg, wob=wob: e.tensor_scalar(wob, stg, PP[:, 188 + cc:189 + cc], 0.0, ALU.mult, ALU.add)),
                 reads=[skey, "PP"], writes=["WOB"])

        def h2tile(ti):
            if ti < 8:
                return XTBf[:, ti * 1024:(ti + 1) * 1024]
            return WINBf[:, (ti - 8) * 1024:(ti - 7) * 1024]

        for ti in range(16):
            XS = WK[3][:, (ti % 2) * 1024:(ti % 2) * 1024 + 1024]
            xkey = "XS%d" % (ti % 2)
            p.dma("sync", (lambda e, ti=ti, XS=XS: e.dma_start(out=XS, in_=x_d[ti * 128:(ti + 1) * 128, :])),
                  writes=[xkey])
            cv(1)
            H2t = h2tile(ti)
            hkey = "XTB" if ti < 8 else "WINB"
            for dh in range(2):
                bl, bc = 4 + dh, 6 + dh
                for c in range(4):
                    p.op(P_, (lambda e, bl=bl, c=c, ti=ti, dh=dh: e.matmul(
                        bank(bl), YB[:, c * T + ti * 128: c * T + ti * 128 + 128],
                        WKb[c // 4][:, (c % 4) * 1024 + dh * 512:(c % 4) * 1024 + dh * 512 + 512],
                        start=(c == 0), stop=(c == 3))), reads=["YB", "WOB"], writes=[bk(bl)])
                for c in range(4, 8):
                    p.op(P_, (lambda e, bc=bc, c=c, ti=ti, dh=dh: e.matmul(
                        bank(bc), YB[:, c * T + ti * 128: c * T + ti * 128 + 128],
                        WKb[c // 4][:, (c % 4) * 1024 + dh * 512:(c % 4) * 1024 + dh * 512 + 512],
                        start=(c == 4), stop=(c == 7))), reads=["YB", "WOB"], writes=[bk(bc)])
                p.op(V, (lambda e, bl=bl, ti=ti, dh=dh, XS=XS, H2t=H2t: e.scalar_tensor_tensor(
                    out=H2t[:, dh * 512:(dh + 1) * 512], in0=bank(bl), scalar=RS2[:, ti:ti + 1],
                    in1=XS[:, dh * 512:(dh + 1) * 512], op0=ALU.mult, op1=ALU.add)),
                    reads=[bk(bl), "SMr", xkey], writes=[hkey])
                p.op(V, (lambda e, bc=bc, ti=ti, dh=dh, H2t=H2t: e.scalar_tensor_tensor(
                    out=H2t[:, dh * 512:(dh + 1) * 512], in0=bank(bc), scalar=RS2[:, 16 + ti:17 + ti],
                    in1=H2t[:, dh * 512:(dh + 1) * 512], op0=ALU.mult, op1=ALU.add)),
                    reads=[bk(bc), "SMr", hkey], writes=[hkey])

        p.barrier()

        if stop_after == "A":
            for ti in range(16):
                p.dma("sync", (lambda e, ti=ti: e.dma_start(out=out_d[ti * 128:(ti + 1) * 128, :], in_=h2tile(ti))),
                      writes=["out%d" % ti])
            p.finish("sync", ["out%d" % ti for ti in range(16)])
            p.emit()
            return nc

        IDXT = YBu[:, 0:2048]
        SKT = YBf[:, 2048:4096]
        G2B = YBf[:, 4096:5120]
        GATET = YBf[:, 6144:8192]
        p.dma("sync", lambda e: e.dma_start(out=G2B, in_=g2_d.partition_broadcast(128)), writes=["G2B"])
        p.dma("sync", lambda e: e.dma_start(out=sub(SKT, 0, [[128, 16], [1, 128]]), in_=skT_d.rearrange("e c k -> c e k")),
              writes=["SKT"])
        WQB = [WKb[4 + dc // 2][:, (dc % 2) * 2048:(dc % 2) * 2048 + 2048] for dc in range(8)]
        for dc in range(8):
            stg = WK[8 + dc % 2][:, 0:2048]
            skey = "WQS%d" % (dc % 2)
            p.dma("sync", (lambda e, dc=dc, stg=stg: e.dma_start(out=stg, in_=wq_d[dc * 128:(dc + 1) * 128, :])), writes=[skey])
            p.op(V, (lambda e, dc=dc, stg=stg: e.tensor_copy(WQB[dc], stg)), reads=[skey], writes=["WQB"])
        for ti in range(16):
            p.op(A, (lambda e, ti=ti: e.activation(out=WK[1][:, 1024:2048], in_=h2tile(ti), func=AF.Square,
                                                   accum_out=SS2[:, ti:ti + 1])),
                 reads=[("h2", ti)], writes=["junkA", ("ss2", ti)])
        p.op(A, lambda e: e.activation(out=RSB, in_=SS2, func=AF.Sqrt, bias=EPSC, scale=1.0 / D),
             reads=[("ss2", ti) for ti in range(16)] + ["SM"], writes=["RSB"])
        p.op(V, lambda e: e.reciprocal(RSB, RSB), reads=["RSB"], writes=["RSB"])

        def reg(i, n=128):
            return WK[2][:, i * 128:i * 128 + n]

        TOP, TOPI, TOPIf = reg(0, 256), reg(2, 256).bitcast(U32), reg(4, 256)
        WRK, WRK2 = reg(6), reg(7, 256)
        TS, POS = reg(9), reg(10).bitcast(U32)
        PAu, PBu = reg(11).bitcast(U32), reg(12).bitcast(U32)
        PAf, PBf = reg(13), reg(14)
        E1, E2 = reg(15), WK[1][:, 0:128]
        EIDX, EX, GATE = WK[1][:, 128:256], WK[1][:, 256:384], WK[1][:, 384:512]
        Zs, RZ = WK[1][:, 512:520], WK[1][:, 520:528]
        C_ = WKf[0]
        OHt = RSTDb[:, 2048:4096]

        def n2tile(slot):
            return RSTDb[:, slot * 1024:(slot + 1) * 1024]

        def compute_n2(ti, slot):
            p.op(V, (lambda e, ti=ti, slot=slot: e.scalar_tensor_tensor(
                out=n2tile(slot), in0=h2tile(ti), scalar=RSB[:, ti:ti + 1], in1=G2B, op0=ALU.mult, op1=ALU.mult)),
                reads=[("h2", ti), "RSB", "G2B"], writes=[("n2", slot)])

        SBUFS = [[WK[8][:, 0:1024], WK[8][:, 1024:2048]], [WK[1][:, 1024:2048], YBf[:, 5120:6144]]]
        def b1_stage1(ti):
            cv(8)
            if lvl < 1:
                return
            slot = ti % 2
            compute_n2(ti, slot)
            N2 = n2tile(slot)
            PT = PS2[0][:, 0:512].bitcast(BF16)
            for dc in range(8):
                p.op(P_, (lambda e, dc=dc, N2=N2, PT=PT: e.transpose(PT[:, dc * 128:(dc + 1) * 128], N2[:, dc * 128:(dc + 1) * 128], identb[:])),
                     reads=[("n2", slot), "identb"], writes=[bk(0)])
            N2T = WKb[3][:, slot * 1024:(slot + 1) * 1024]
            p.op(A, (lambda e, N2T=N2T, PT=PT: e.copy(out=N2T, in_=PT)), reads=[bk(0)], writes=[("n2t", slot)])
            if lvl < 2:
                return
            QSB = WKf[9]
            Sh = SBUFS[ti % 2]
            for half in range(2):
                QP = PS2[1]
                for e8 in range(8):
                    ec = half * 8 + e8
                    for dc in range(8):
                        p.op(P_, (lambda e, e8=e8, ec=ec, dc=dc, N2T=N2T, QP=QP: e.matmul(
                            QP[:, e8 * 128:(e8 + 1) * 128], WQB[dc][:, ec * 128:(ec + 1) * 128],
                            N2T[:, dc * 128:(dc + 1) * 128], start=(dc == 0), stop=(dc == 7))),
                            reads=["WQB", ("n2t", slot)], writes=["QP"])
                p.op(A, (lambda e, half=half, QP=QP: e.copy(out=QSB[:, half * 1024:(half + 1) * 1024], in_=QP[:, :])),
                     reads=["QP"], writes=[("qsb", half)])
                SP = PS2[2 + half]
                for e8 in range(8):
                    ec = half * 8 + e8
                    p.op(P_, (lambda e, e8=e8, ec=ec, SP=SP: e.matmul(
                        SP[:, e8 * 128:(e8 + 1) * 128], QSB[:, ec * 128:(ec + 1) * 128], SKT[:, ec * 128:(ec + 1) * 128],
                        start=True, stop=True)), reads=[("qsb", half), "SKT"], writes=[("SP", half)])
                p.op(A, (lambda e, half=half, SP=SP, Sh=Sh: e.copy(out=Sh[half], in_=SP[:, :])),
                     reads=[("SP", half)], writes=[("S", ti % 2, half)])
        def b1_stage2(ti):
            Sh = SBUFS[ti % 2]
            if lvl < 3:
                return
            for g in range(16):
                Sg = Sh[g // 8][:, (g % 8) * 128:(g % 8) * 128 + 128]
                sk = ("S", ti % 2, g // 8)
                t0, t1 = TOP[:, g * 16:g * 16 + 8], TOP[:, g * 16 + 8:g * 16 + 16]
                i0, i1 = TOPI[:, g * 16:g * 16 + 8], TOPI[:, g * 16 + 8:g * 16 + 16]
                p.op(V, (lambda e, Sg=Sg, t0=t0: e.max(out=t0, in_=Sg)), reads=[sk], writes=["TOP"])
                p.op(V, (lambda e, Sg=Sg, t0=t0, i0=i0: e.max_index(out=i0, in_max=t0, in_values=Sg)), reads=[sk, "TOP"], writes=["TOPI"])
                p.op(V, (lambda e, Sg=Sg, t0=t0: e.match_replace(out=WRK, in_to_replace=t0, in_values=Sg, imm_value=-1e30)),
                     reads=[sk, "TOP"], writes=["WRK"])
                p.op(V, (lambda e, t1=t1: e.max(out=t1, in_=WRK)), reads=["WRK"], writes=["TOP"])
                p.op(V, (lambda e, t1=t1, i1=i1: e.max_index(out=i1, in_max=t1, in_values=WRK)), reads=["WRK", "TOP"], writes=["TOPI"])
            p.op(V, lambda e: e.tensor_copy(TOPIf, TOPI), reads=["TOPI"], writes=["TOPIf"])
            if lvl < 4:
                return
            p.op(V, lambda e: e.tensor_tensor(sub(C_, 0, [[256, 8], [16, 16], [1, 16]]),
                                              sub(TOP, 0, [[32, 8], [1, 16], [0, 16]]),
                                              sub(TOP, 16, [[32, 8], [0, 16], [1, 16]]), ALU.add),
                 reads=["TOP"], writes=["C"])
            for h in range(8):
                Ch = C_[:, h * 256:(h + 1) * 256]
                t0, t1 = TS[:, h * 16:h * 16 + 8], TS[:, h * 16 + 8:h * 16 + 16]
                i0, i1 = POS[:, h * 16:h * 16 + 8], POS[:, h * 16 + 8:h * 16 + 16]
                p.op(V, (lambda e, Ch=Ch, t0=t0: e.max(out=t0, in_=Ch)), reads=["C"], writes=["TS"])
                p.op(V, (lambda e, Ch=Ch, t0=t0, i0=i0: e.max_index(out=i0, in_max=t0, in_values=Ch)), reads=["C", "TS"], writes=["POS"])
                p.op(V, (lambda e, Ch=Ch, t0=t0: e.match_replace(out=WRK2, in_to_replace=t0, in_values=Ch, imm_value=-1e30)),
                     reads=["C", "TS"], writes=["WRK2"])
                p.op(V, (lambda e, t1=t1: e.max(out=t1, in_=WRK2)), reads=["WRK2"], writes=["TS"])
                p.op(V, (lambda e, t1=t1, i1=i1: e.max_index(out=i1, in_max=t1, in_values=WRK2)), reads=["WRK2", "TS"], writes=["POS"])
            if lvl < 5:
                return
            p.op(V, lambda e: e.tensor_single_scalar(PAu, POS, 4, ALU.logical_shift_right), reads=["POS"], writes=["PAu"])
            p.op(V, lambda e: e.tensor_single_scalar(PBu, POS, 15, ALU.bitwise_and), reads=["POS"], writes=["PBu"])
            p.op(V, lambda e: e.tensor_copy(PAf, PAu), reads=["PAu"], writes=["PAf"])
            p.op(V, lambda e: e.tensor_copy(PBf, PBu), reads=["PBu"], writes=["PBf"])
            for (PXf, pk, off, Eo, ek) in ((PAf, "PAf", 0, E1, "E1"), (PBf, "PBf", 16, E2, "E2")):
                p.op(V, (lambda e, PXf=PXf: e.tensor_tensor(sub(OHt, 0, [[256, 8], [16, 16], [1, 16]]),
                                                            sub(IOTA, 0, [[0, 8], [0, 16], [1, 16]]),
                                                            sub(PXf, 0, [[16, 8], [1, 16], [0, 16]]), ALU.is_equal)),
                     reads=[pk, "SM"], writes=["OH"])
                p.op(V, (lambda e, off=off: e.tensor_tensor(sub(OHt, 0, [[256, 8], [16, 16], [1, 16]]),
                                                            sub(OHt, 0, [[256, 8], [16, 16], [1, 16]]),
                                                            sub(TOPIf, off, [[32, 8], [0, 16], [1, 16]]), ALU.mult)),
                     reads=["OH", "TOPIf"], writes=["OH"])
                p.op(V, (lambda e, Eo=Eo: e.tensor_reduce(out=Eo, in_=sub(OHt, 0, [[16, 128], [1, 16]]), axis=AX.X, op=ALU.add)),
                     reads=["OH"], writes=[ek])
            p.op(V, lambda e: e.scalar_tensor_tensor(out=EIDX, in0=E1, scalar=128.0, in1=E2, op0=ALU.mult, op1=ALU.add),
                 reads=["E1", "E2"], writes=["EIDX"])
            if lvl < 6:
                return
            p.op(V, lambda e: e.tensor_tensor(sub(EX, 0, [[16, 8], [1, 16]]), sub(TS, 0, [[16, 8], [1, 16]]),
                                              sub(TS, 0, [[16, 8], [0, 16]]), ALU.subtract), reads=["TS"], writes=["EX"])
            p.op(A, lambda e: e.activation(out=EX, in_=EX, func=AF.Exp), reads=["EX"], writes=["EX"])
            p.op(V, lambda e: e.tensor_reduce(out=Zs, in_=sub(EX, 0, [[16, 8], [1, 16]]), axis=AX.X, op=ALU.add), reads=["EX"], writes=["Zs"])
            p.op(V, lambda e: e.reciprocal(RZ, Zs), reads=["Zs"], writes=["RZ"])
            p.op(V, lambda e: e.tensor_tensor(sub(GATE, 0, [[16, 8], [1, 16]]), sub(EX, 0, [[16, 8], [1, 16]]),
                                              sub(RZ, 0, [[1, 8], [0, 16]]), ALU.mult), reads=["EX", "RZ"], writes=["GATE"])
            if lvl < 7:
                return
            p.op(P_, lambda e: e.matmul(PS2[0][:, 512:640], EIDX, ident[:], start=True, stop=True), reads=["EIDX", "ident"], writes=[bk(1)])
            p.op(P_, lambda e: e.matmul(PS2[0][:, 640:768], GATE, ident[:], start=True, stop=True), reads=["GATE", "ident"], writes=[bk(1)])
            p.op(V, (lambda e, ti=ti: e.tensor_copy(IDXT[:, ti * 128:(ti + 1) * 128], PS2[0][:, 512:640])), reads=[bk(1)], writes=[("idxt", ti)])
            p.op(A, (lambda e, ti=ti: e.copy(out=GATET[:, ti * 128:(ti + 1) * 128], in_=PS2[0][:, 640:768])), reads=[bk(1)], writes=[("gatet", ti)])

        b1_stage1(0)
        for ti in range(16):
            if ti + 1 < 16:
                b1_stage1(ti + 1)
            b1_stage2(ti)
        cv(NPIECE)
        p.barrier()
        if stop_after == "B1":
            for q_, (c0) in enumerate((0, 1024, 6144, 7168)):
                p.dma("sync", (lambda e, q_=q_, c0=c0: e.dma_start(out=out_d[q_ * 128:(q_ + 1) * 128, :], in_=YBf[:, c0:c0 + 1024])),
                      writes=["out%d" % q_])
            p.finish("sync", ["out%d" % q_ for q_ in range(4)])
            p.emit()
            return nc

        GFB = WK[9][:, 1024:2048]
        OUTT = WK[9][:, 0:1024]
        ZC = WKb[8][:, 0:256]
        ACTM = WK[8][:, 256:384]
        GA = WK[8][:, 384:512]
        LTG = [WKb[8][:, 1024:2048], WKb[8][:, 2048:3072]]
        JUNK = WKb[8][:, 3072:4096]
        JUNK2 = WKb[8][:, 3072:4096]
        p.dma("sync", lambda e: e.dma_start(out=GFB, in_=gf_d.partition_broadcast(128)), writes=["GFB"])
        p.op(V, lambda e: e.memset(ZC, 0.0), writes=["ZC"])
        p.op(V, lambda e: e.memset(ZC[:, 128:129], 1.0), reads=["ZC"], writes=["ZC"])
        NS = 16
        GS = 8
        SL = [WKb[s_ // 2][:, (s_ % 2) * 2048:(s_ % 2) * 2048 + 2048] for s_ in range(16)]
        CVIb = CVI[:].bitcast(BF16)
        SL += [CVIb[:, 0:2048], CVIb[:, 2048:4096], CVO[:, 0:2048]]
        NS = len(SL)
        LAG = 13
        XSB = [RSTDb[:, 2048:3072], RSTDb[:, 3072:4096]]
        PRB = [YB[:, 10240:11264], YB[:, 11264:12288]]
        NTOK = 16 * 128
        for step in range(NTOK + LAG):
            nu, nv = step, step - LAG
            if nu < NTOK:
                ti, t, s_ = nu // 128, nu % 128, nu % NS
                if t == 0:
                    compute_n2(ti, ti % 2)
                N2 = n2tile(ti % 2)
                XBP = PS2[nu % 3]
                xk = ("xbp", nu % 3)
                p.dma(G, (lambda e, s_=s_, nu=nu: e.indirect_dma_start(
                    out=SL[s_], out_offset=None, in_=puv_d,
                    in_offset=bass.IndirectOffsetOnAxis(ap=IDXT[:, nu:nu + 1], axis=0))),
                    reads=[("idxt", ti)], writes=[("sl", s_)], stream=("g", s_))
                for half in range(2):
                    p.op(P_, (lambda e, t=t, half=half, XBP=XBP, N2=N2: e.matmul(
                        XBP[:, half * 512:(half + 1) * 512], identb[:, t:t + 1].to_broadcast([128, 128]),
                        N2[:, half * 512:(half + 1) * 512], start=True, stop=True)),
                        reads=["identb", ("n2", ti % 2)], writes=[xk])
            mm = step - 4
            if 0 <= mm < NTOK and mm % GS == GS - 1:
                ti_m, g0 = mm // 128, (mm % 128) - (GS - 1)
                gk_m = ("ga", (mm // GS) % 16)
                p.op(V, (lambda e, g0=g0, ti_m=ti_m: e.tensor_tensor(GA[:, g0:g0 + GS], GA[:, g0:g0 + GS],
                                                                     GATET[:, ti_m * 128 + g0: ti_m * 128 + g0 + GS], ALU.mult)),
                     reads=[gk_m, ("gatet", ti_m)], writes=[gk_m])
                gsl = (mm // GS) % 2
                p.op(V, (lambda e, g0=g0, gsl=gsl: e.tensor_tensor(
                    sub(LTG[gsl], 0, [[128, GS], [1, 128]]),
                    sub(ZC, 128 - g0, [[-1, GS], [1, 128]]),
                    sub(GA, g0, [[1, GS], [0, 128]]), ALU.mult)),
                    reads=["ZC", gk_m], writes=[("ltg", gsl)])
            if nv >= 0:
                ti, t, s_ = nv // 128, nv % 128, nv % NS
                OP = PS2[3]
                ok = ("op", 0)
                gsl_v = (nv // GS) % 2
                lt = LTG[gsl_v][:, (nv % GS) * 128:(nv % GS) * 128 + 128]
                for half in range(2):
                    p.op(P_, (lambda e, s_=s_, t=t, half=half, lt=lt, OP=OP: e.matmul(
                        OP[:, half * 512:(half + 1) * 512], lt, SL[s_][:, 1024 + half * 512:1024 + (half + 1) * 512],
                        start=(t == 0), stop=(t == 127))),
                        reads=[("ltg", gsl_v), ("sl", s_)], writes=[ok])
            if nu < NTOK:
                ti, t, s_ = nu // 128, nu % 128, nu % NS
                if nu % 2 == 0:
                    p.op(V, (lambda e, s_=s_, t=t, XBP=XBP: e.scalar_tensor_tensor(
                        out=JUNK, in0=SL[s_][:, 0:1024], scalar=1.0, in1=XBP[:, :], op0=ALU.mult, op1=ALU.mult,
                        accum_out=ACTM[:, t:t + 1])),
                        reads=[("sl", s_), xk], writes=[("actm", t)])
                else:
                    q_ = (nu // 2) % 2
                    p.op(A, (lambda e, q_=q_, XBP=XBP: e.copy(out=XSB[q_], in_=XBP[:, :])), reads=[xk], writes=[("xsb", q_)])
                    p.op(V, (lambda e, q_=q_, s_=s_: e.tensor_tensor(PRB[q_], SL[s_][:, 0:1024], XSB[q_], ALU.mult)),
                         reads=[("sl", s_), ("xsb", q_)], writes=[("prb", q_)])
            ao = step - 2
            if 0 <= ao < NTOK and ao % 2 == 1:
                q_ = (ao // 2) % 2
                t_ao = ao % 128
                p.op(A, (lambda e, q_=q_, t_ao=t_ao: e.activation(out=JUNK2, in_=PRB[q_], func=AF.Identity,
                                                                  accum_out=ACTM[:, t_ao:t_ao + 1])),
                     reads=[("prb", q_)], writes=[("actm", t_ao)])
                if t_ao % GS == GS - 1:
                    g0 = t_ao - (GS - 1)
                    gk = ("ga", (ao // GS) % 16)
                    p.op(A, (lambda e, g0=g0: e.activation(out=GA[:, g0:g0 + GS], in_=ACTM[:, g0:g0 + GS], func=AF.Gelu_apprx_tanh)),
                         reads=[("actm", tt_) for tt_ in range(g0, g0 + GS)], writes=[gk])
            if nv >= 0 and nv % 128 == 127:
                ti = nv // 128
                OP = PS2[3]
                ok = ("op", 0)
                p.op(V, (lambda e, ti=ti, OP=OP: e.tensor_tensor(OUTT, h2tile(ti), OP[:, :], ALU.add)),
                     reads=[("h2", ti), ok], writes=["OUTT"])
                p.op(A, lambda e: e.activation(out=JUNK2, in_=OUTT, func=AF.Square, accum_out=SS3), reads=["OUTT"], writes=["JUNK2", "SS3"])
                p.op(A, lambda e: e.activation(out=R3, in_=SS3, func=AF.Sqrt, bias=EPSC, scale=1.0 / D), reads=["SS3", "SM"], writes=["R3"])
                p.op(V, lambda e: e.reciprocal(R3, R3), reads=["R3"], writes=["R3"])
                p.op(V, lambda e: e.scalar_tensor_tensor(out=OUTT, in0=OUTT, scalar=R3, in1=GFB, op0=ALU.mult, op1=ALU.mult),
                     reads=["OUTT", "R3", "GFB"], writes=["OUTT"])
                p.dma("sync", (lambda e, ti=ti: e.dma_start(out=out_d[ti * 128:(ti + 1) * 128, :], in_=OUTT)),
                      reads=["OUTT"], writes=["out%d" % ti])
        p.finish("sync", ["out%d" % ti for ti in range(16)])
        p.emit()
    return nc


def make_in_maps(x, mix_norm_g, w_in, lru_conv_w, lru_conv_b, lru_w_rg, lru_b_rg, lru_w_ig, lru_b_ig,
                 lru_lambda, conf_conv_w, conf_conv_b, conf_norm_g, conf_norm_b, beta_lru, beta_conv,
                 w_out, ffn_norm_g, peer_w_q, peer_sub_keys, peer_u, peer_v, final_norm_g):
    f = lambda a: np.ascontiguousarray(np.asarray(a, dtype=np.float32))

    def cols(v):
        return f(v).reshape(-1, 128).T

    pp = np.concatenate([
        cols(mix_norm_g[0]),
        np.concatenate([cols(lru_conv_w[0][k]) for k in range(4)], axis=1),
        cols(lru_conv_b[0]),
        cols(lru_b_rg[0][0]), cols(lru_b_ig[0][0]), cols(lru_b_rg[0][1]), cols(lru_b_ig[0][1]),
        cols(lru_lambda[0][0]), cols(lru_lambda[0][1]),
        np.concatenate([cols(conf_conv_w[0][k]) for k in range(31)], axis=1),
        cols(conf_conv_b[0]), cols(conf_norm_g[0]), cols(conf_norm_b[0]),
        cols(beta_lru[0]), cols(beta_conv[0]),
    ], axis=1)
    pp = f(pp)
    assert pp.shape == (128, NPP)
    wg = f(np.stack([lru_w_rg[0][0], lru_w_ig[0][0], lru_w_rg[0][1], lru_w_ig[0][1]], axis=0))
    skT = f(np.transpose(np.asarray(peer_sub_keys[0]), (0, 1, 3, 2)).reshape(16, 128, 128))
    shared = {
        "w_in": f(w_in[0]), "w_out": f(w_out[0]), "w_q": f(peer_w_q[0]), "skT": skT,
        "pu": f(peer_u[0]), "pv": f(peer_v[0]), "pp": pp, "wg": wg,
        "g2": f(ffn_norm_g[0]).reshape(1, D), "gf": f(final_norm_g).reshape(1, D),
    }
    xs = np.asarray(x, dtype=np.float32)
    maps = []
    for b in range(xs.shape[0]):
        m = dict(shared)
        m["x"] = f(xs[b])
        m["xT"] = f(xs[b].T)
        maps.append(m)
    return maps


_NC = None


def kernel(**inputs):
    global _NC
    in_maps = make_in_maps(**inputs)
    if _NC is None:
        _NC = build()
    res = run_bass_kernel_spmd(_NC, in_maps, core_ids=list(range(len(in_maps))))
    return np.stack([np.asarray(r["out"], dtype=np.float32) for r in res.results], axis=0)
```

```python
import numpy as np
from contextlib import ExitStack
import concourse.bass as bass
import concourse.mybir as mybir
from concourse.bass_utils import run_bass_kernel_spmd

F32 = mybir.dt.float32
BF16 = mybir.dt.bfloat16
U32 = mybir.dt.uint32
I32 = mybir.dt.int32
AF = mybir.ActivationFunctionType
ALU = mybir.AluOpType
AX = mybir.AxisListType

T = 2048
D = 1024
EPS = 1e-6
NPP = 196
WKC = 2056


class Prog:
    def __init__(self, nc):
        self.nc = nc
        self.ops = {e: [] for e in ("sync", "scalar", "vector", "gpsimd", "tensor")}
        self.cnt = {}
        self.waited = {e: {} for e in self.ops}
        self.last_w = {}
        self.readers = {}
        self.semkeys = []

    def _semkey(self, k):
        if k not in self.cnt:
            self.cnt[k] = 0
            self.semkeys.append(k)
        return k

    def _deps(self, eng, reads, writes, own_key, skip_same=False):
        need = {}

        def add(k, v):
            if skip_same and k == own_key:
                return
            if need.get(k, 0) < v:
                need[k] = v

        for b in reads:
            d = self.last_w.get(b)
            if d is not None:
                add(*d)
        for b in writes:
            d = self.last_w.get(b)
            if d is not None:
                add(*d)
            for k, v in self.readers.get(b, {}).items():
                add(k, v)
        waits = []
        for k, v in need.items():
            if self.waited[eng].get(k, 0) < v:
                self.waited[eng][k] = v
                waits.append((k, v))
        return waits

    def _commit(self, reads, writes, key, val):
        for b in reads:
            r = self.readers.setdefault(b, {})
            if r.get(key, 0) < val:
                r[key] = val
        for b in writes:
            self.last_w[b] = (key, val)
            self.readers[b] = {}

    @staticmethod
    def _ispsum(k):
        return k == "QP" or (isinstance(k, tuple) and k[0] in ("pb", "SP", "xbp", "op"))

    def op(self, eng, fn, reads=(), writes=()):
        ps = [r for r in reads if self._ispsum(r)]
        if ps:
            reads = [r for r in reads if not self._ispsum(r)]
            writes = list(writes) + ps
        key = self._semkey(eng)
        waits = self._deps(eng, reads, writes, key, skip_same=(eng == "tensor"))
        self.cnt[key] += 1
        self.ops[eng].append((waits, fn, key, 1))
        self._commit(reads, writes, key, self.cnt[key])

    def dma(self, queue, fn, reads=(), writes=(), stream=None):
        if stream is None:
            stream = tuple(writes)
        key = self._semkey(("dma", stream))
        waits = self._deps(queue, reads, writes, key)
        self.cnt[key] += 16
        self.ops[queue].append((waits, fn, key, 16))
        self._commit(reads, writes, key, self.cnt[key])

    def barrier(self):
        for e in self.ops:
            waits = []
            for k in self.semkeys:
                v = self.cnt[k]
                if v > 0 and self.waited[e].get(k, 0) < v:
                    self.waited[e][k] = v
                    waits.append((k, v))
            self.ops[e].append((waits, None, None, 0))
        self.last_w = {}
        self.readers = {}

    def finish(self, eng, bufs):
        waits = self._deps(eng, bufs, (), None)
        self.ops[eng].append((waits, None, None, 0))

    def emit(self):
        nc = self.nc
        with ExitStack() as st:
            sems = {}
            for i, k in enumerate(self.semkeys):
                sems[k] = st.enter_context(nc.semaphore("s%d" % i))
            block = st.enter_context(nc.Block())

            def run(engname):
                def body(e):
                    for waits, fn, key, inc in self.ops[engname]:
                        for k, v in waits:
                            e.wait_ge(sems[k], v)
                        if fn is not None:
                            fn(e).then_inc(sems[key], inc)
                return body

            block.sync(run("sync"))
            block.scalar(run("scalar"))
            block.vector(run("vector"))
            block.gpsimd(run("gpsimd"))
            block.tensor(run("tensor"))


def sub(ap, off, dims):
    return bass.AP(ap.tensor, ap.offset + off, [list(ap.ap[0])] + [list(d) for d in dims])


def build(stop_after=None, lvl=99):
    nc = bass.Bass("TRN2", target_bir_lowering=False)

    def dram(name, shape, dt=F32, kind="ExternalInput"):
        return nc.dram_tensor(name, shape, dt, kind=kind).ap()

    xT_d = dram("xT", [D, T])
    x_d = dram("x", [T, D])
    win_d = dram("w_in", [D, 2048])
    wout_d = dram("w_out", [D, D])
    wq_d = dram("w_q", [D, 2048])
    skT_d = dram("skT", [16, 128, 128])
    pu_d = dram("pu", [16384, D])
    pv_d = dram("pv", [16384, D])
    pp_d = dram("pp", [128, NPP])
    wg_d = dram("wg", [4, 8, 64, 64])
    g2_d = dram("g2", [1, D])
    gf_d = dram("gf", [1, D])
    out_d = dram("out", [T, D], kind="ExternalOutput")
    puv_d = dram("puv", [16384, 2 * D], BF16, kind="Internal")

    with ExitStack() as st:
        def sb(name, shape, dt):
            return st.enter_context(nc.sbuf_tensor(name, shape, dt))

        WK = [sb("wk%d" % i, [128, WKC], F32) for i in range(10)]
        XTB = sb("xtb", [128, 8 * T], BF16)
        WINB = sb("winb", [128, 8 * 2048], BF16)
        YB = sb("yb", [128, 8 * T], BF16)
        RSTD = sb("rstd", [128, T], F32)
        PP = sb("pp_sb", [128, NPP], F32)
        BDB = sb("bdb", [128, 2048], BF16)
        ident = sb("ident", [128, 128], F32)
        identb = sb("identb", [128, 128], BF16)
        ones = sb("ones", [128, 128], F32)
        GM = sb("gm", [128, 128], F32)
        SM = sb("sm", [128, 256], F32)
        CVI = sb("cvi", [128, 2048], F32)
        CVO = sb("cvo", [128, 2048], BF16)
        PS2 = [st.enter_context(nc.psum_tensor("ps%d" % i, [128, 1024], F32)) for i in range(4)]

        WKf = [w[:] for w in WK]
        WKb = [w[:].bitcast(BF16) for w in WK]
        XTBf = XTB[:].bitcast(F32)
        WINBf = WINB[:].bitcast(F32)
        YBf = YB[:].bitcast(F32)
        YBu = YB[:].bitcast(U32)
        RSTDb = RSTD[:].bitcast(BF16)

        def bank(b):
            return PS2[b // 2][:, (b % 2) * 512:(b % 2) * 512 + 512]

        def bk(b):
            return ("pb", b)

        CL = SM[:, 0:8]
        CL2 = SM[:, 8:16]
        EPSC = SM[:, 16:17]
        ONEC = SM[:, 17:18]
        LSG = SM[:, 18:26]
        RS2 = SM[:, 32:64]
        SS2 = SM[:, 64:80]
        RSB = SM[:, 80:96]
        SS3 = SM[:, 96:97]
        R3 = SM[:, 97:98]
        IOTA = SM[:, 128:144]
        IOTAi = SM[:, 144:160].bitcast(I32)

        p = Prog(nc)
        V, A, G, P_ = "vector", "scalar", "gpsimd", "tensor"

        cv_src = [pu_d.rearrange("(p r) d -> p (r d)", p=128), pv_d.rearrange("(p r) d -> p (r d)", p=128)]
        puv_v = puv_d.rearrange("(p r) d -> p r d", p=128)
        NPIECE = 256
        cvn = [0]

        def cv_in(i):
            if i >= NPIECE:
                return
            tb, j, sl = i // 128, i % 128, i % 2
            p.dma(G, (lambda e, tb=tb, j=j, sl=sl: e.dma_start(out=CVI[:, sl * 1024:(sl + 1) * 1024],
                                                             in_=cv_src[tb][:, j * 1024:(j + 1) * 1024])),
                  writes=[("cvi", sl)], stream=("cvi", sl))

        def cv(n):
            for _ in range(n):
                i = cvn[0]
                if i >= NPIECE:
                    return
                if i == 0:
                    cv_in(0)
                    cv_in(1)
                tb, j, sl = i // 128, i % 128, i % 2
                p.op(G, (lambda e, sl=sl: e.tensor_copy(CVO[:, sl * 1024:(sl + 1) * 1024], CVI[:, sl * 1024:(sl + 1) * 1024])),
                     reads=[("cvi", sl)], writes=[("cvo", sl)])
                p.dma(G, (lambda e, tb=tb, j=j, sl=sl: e.dma_start(out=puv_v[:, j, tb * 1024:(tb + 1) * 1024],
                                                                 in_=CVO[:, sl * 1024:(sl + 1) * 1024])),
                      reads=[("cvo", sl)], writes=[("cvd", i)], stream=("cvo", sl))
                cv_in(i + 2)
                cvn[0] += 1

        p.dma("sync", lambda e: e.dma_start(out=PP[:], in_=pp_d), writes=["PP"])
        p.op(G, lambda e: e.memset(ident[:], 0.0), writes=["ident"])
        p.op(G, lambda e: e.affine_select(out=ident[:], in_=ident[:], pattern=[[-1, 128]],
                                          compare_op=ALU.not_equal, fill=1.0, base=0,
                                          channel_multiplier=1), reads=["ident"], writes=["ident"])
        p.op(V, lambda e: e.tensor_copy(identb[:], ident[:]), reads=["ident"], writes=["identb"])
        p.op(G, lambda e: e.memset(ones[:], 1.0), writes=["ones"])
        p.op(G, lambda e: e.memset(GM[:], 0.0), writes=["GM"])
        p.op(G, lambda e: e.memset(GM[0:64, 0:64], 1.0 / 64), reads=["GM"], writes=["GM"])
        p.op(G, lambda e: e.memset(GM[64:128, 64:128], 1.0 / 64), reads=["GM"], writes=["GM"])
        p.op(G, lambda e: e.memset(SM[:], 0.0), writes=["SM"])
        p.op(G, lambda e: e.memset(EPSC, EPS), reads=["SM"], writes=["SM"])
        p.op(G, lambda e: e.memset(ONEC, 1.0), reads=["SM"], writes=["SM"])
        p.op(G, lambda e: e.iota(IOTAi, pattern=[[1, 16]], base=0, channel_multiplier=0),
             reads=["SM"], writes=["SM"])
        p.op(V, lambda e: e.tensor_copy(IOTA, IOTAi), reads=["SM"], writes=["SM"])
        p.op(G, lambda e: e.memset(WKf[0], 0.0), writes=["WK0"])
        p.op(G, lambda e: e.memset(WKf[3], 0.0), writes=["WK3"])
        p.op(G, lambda e: e.memset(WKf[9], 0.0), writes=["WK9"])
        for kind in range(4):
            for c in range(4):
                for hl in range(2):
                    col = (kind * 4 + c) * 128 + hl * 64
                    p.dma("sync", (lambda e, kind=kind, c=c, hl=hl, col=col: e.dma_start(
                        out=WK[9][hl * 64:(hl + 1) * 64, col:col + 64], in_=wg_d[kind, 2 * c + hl])),
                        writes=["WK9"])
        p.op(A, lambda e: e.copy(out=BDB[:], in_=WK[9][:, 0:2048]), reads=["WK9"], writes=["BDB"])
        p.op(A, lambda e: e.activation(out=LSG, in_=PP[:, 44:52], func=AF.Sigmoid), reads=["PP", "SM"], writes=["SM"])
        p.op(A, lambda e: e.activation(out=LSG, in_=LSG, func=AF.Ln), reads=["SM"], writes=["SM"])
        p.op(V, lambda e: e.tensor_scalar(CL, LSG, 8.0, None, ALU.mult), reads=["SM"], writes=["SM"])
        p.op(V, lambda e: e.tensor_scalar(CL2, LSG, 16.0, None, ALU.mult), reads=["SM"], writes=["SM"])

        for c in range(8):
            stg = 1 + c % 2
            sq = 4 + c % 2
            p.dma("sync", (lambda e, c=c, stg=stg: e.dma_start(out=WK[stg][:, 0:T], in_=xT_d[c * 128:(c + 1) * 128, :])),
                  writes=["WK%d" % stg])
            p.op(A, (lambda e, stg=stg, sq=sq: e.activation(out=WK[sq][:, 0:T], in_=WK[stg][:, 0:T], func=AF.Square)),
                 reads=["WK%d" % stg], writes=["WK%d" % sq])
            p.op(V, (lambda e, c=c, stg=stg: e.tensor_copy(XTB[:, c * T:(c + 1) * T], WK[stg][:, 0:T])),
                 reads=["WK%d" % stg], writes=["XTB"])
            for tt in range(4):
                p.op(P_, (lambda e, c=c, sq=sq, tt=tt: e.matmul(bank(tt), ones[:], WK[sq][:, tt * 512:(tt + 1) * 512],
                                                                 start=(c == 0), stop=(c == 7))),
                     reads=["ones", "WK%d" % sq], writes=[bk(tt)])
        cv(16)
        for tt in range(4):
            p.op(A, (lambda e, tt=tt: e.activation(out=RSTD[:, tt * 512:(tt + 1) * 512], in_=bank(tt), func=AF.Sqrt,
                                                   bias=EPSC, scale=1.0 / D)),
                 reads=[bk(tt), "SM"], writes=["RSTD"])
        p.op(V, lambda e: e.reciprocal(RSTD[:], RSTD[:]), reads=["RSTD"], writes=["RSTD"])
        for c in range(8):
            stg = 6 + c % 2
            p.dma("sync", (lambda e, c=c, stg=stg: e.dma_start(out=WK[stg][:, 0:2048], in_=win_d[c * 128:(c + 1) * 128, :])),
                  writes=["WK%d" % stg])
            p.op(V, (lambda e, c=c, stg=stg: e.tensor_scalar(WINB[:, c * 2048:(c + 1) * 2048], WK[stg][:, 0:2048],
                                                             PP[:, c:c + 1], 0.0, ALU.mult, ALU.add)),
                 reads=["WK%d" % stg, "PP"], writes=["WINB"])

        zcnt = [0]

        def zchunk(f, dst, dkey, doff):
            for tt in range(4):
                b = 4 + zcnt[0] % 2
                zcnt[0] += 1
                for dc in range(8):
                    p.op(P_, (lambda e, b=b, dc=dc, tt=tt, f=f: e.matmul(
                        bank(b), WINB[:, dc * 2048 + f * 128: dc * 2048 + f * 128 + 128],
                        XTB[:, dc * T + tt * 512: dc * T + tt * 512 + 512], start=(dc == 0), stop=(dc == 7))),
                        reads=["WINB", "XTB"], writes=[bk(b)])
                p.op(V, (lambda e, b=b, tt=tt, dst=dst, doff=doff: e.tensor_tensor(
                    dst[:, doff + tt * 512: doff + tt * 512 + 512], bank(b), RSTD[:, tt * 512:(tt + 1) * 512], ALU.mult)),
                    reads=[bk(b), "RSTD"], writes=[dkey])

        gcnt = [0]

        def gbank():
            b = 6 + gcnt[0] % 2
            gcnt[0] += 1
            return b

        def sumsq_cols(src, skey, colbase):
            for ti in range(16):
                p.op(P_, (lambda e, ti=ti, src=src, colbase=colbase: e.matmul(
                    bank(0)[:, colbase + ti: colbase + ti + 1], src[:, ti * 128:(ti + 1) * 128], ones[:, 0:1],
                    start=True, stop=True)), reads=[skey, "ones"], writes=[bk(0)])

        for c in range(4):
            XL, XC, TMP, XCBt = WKf[0], WKf[1], WKf[6], WKb[7]
            zchunk(c, XL, "WK0", 2)
            p.op(V, (lambda e, c=c: e.tensor_scalar(XC[:, 0:T], XL[:, 0:T], PP[:, 8 + c:9 + c], PP[:, 24 + c:25 + c],
                                                    ALU.mult, ALU.add)), reads=["WK0", "PP"], writes=["WK1"])
            for k in range(1, 4):
                p.op(V, (lambda e, c=c, k=k: e.scalar_tensor_tensor(
                    out=XC[:, 0:T], in0=XL[:, k:k + T], scalar=PP[:, 8 + k * 4 + c: 9 + k * 4 + c], in1=XC[:, 0:T],
                    op0=ALU.mult, op1=ALU.add)), reads=["WK0", "WK1", "PP"], writes=["WK1"])
            p.op(A, lambda e: e.copy(out=XCBt[:, 0:T], in_=XC[:, 0:T]), reads=["WK1"], writes=["WK7"])
            for kind in range(4):
                Gt = WKf[2 + kind]
                for tt in range(4):
                    b = gbank()
                    p.op(P_, (lambda e, b=b, kind=kind, c=c, tt=tt: e.matmul(
                        bank(b), BDB[:, (kind * 4 + c) * 128:(kind * 4 + c) * 128 + 128],
                        XCBt[:, tt * 512:(tt + 1) * 512], start=True, stop=True)),
                        reads=["BDB", "WK7"], writes=[bk(b)])
                    p.op(A, (lambda e, b=b, kind=kind, c=c, tt=tt, Gt=Gt: e.activation(
                        out=Gt[:, tt * 512:(tt + 1) * 512], in_=bank(b), func=AF.Sigmoid,
                        bias=PP[:, 28 + kind * 4 + c: 29 + kind * 4 + c])),
                        reads=[bk(b), "PP"], writes=["WK%d" % (2 + kind)])
            for d_ in range(2):
                R, I_, H = WKf[2 + 2 * d_], WKf[3 + 2 * d_], WKf[8 + d_]
                rk, ik, hk = "WK%d" % (2 + 2 * d_), "WK%d" % (3 + 2 * d_), "WK%d" % (8 + d_)
                col = d_ * 4 + c
                p.op(A, (lambda e, R=R, col=col: e.activation(out=TMP[:, 0:T], in_=R[:, 0:T], func=AF.Exp,
                                                               scale=CL2[:, col:col + 1])),
                     reads=[rk, "SM"], writes=["WK6"])
                p.op(A, (lambda e: e.activation(out=TMP[:, 0:T], in_=TMP[:, 0:T], func=AF.Sqrt, bias=ONEC, scale=-1.0)),
                     reads=["WK6", "SM"], writes=["WK6"])
                p.op(A, (lambda e, R=R, col=col: e.activation(out=R[:, 0:T], in_=R[:, 0:T], func=AF.Exp,
                                                               scale=CL[:, col:col + 1])),
                     reads=[rk, "SM"], writes=[rk])
                p.op(V, (lambda e, I_=I_: e.tensor_tensor(I_[:, 0:T], I_[:, 0:T], XC[:, 0:T], ALU.mult)),
                     reads=[ik, "WK1"], writes=[ik])
                p.op(V, (lambda e, I_=I_: e.tensor_tensor(I_[:, 0:T], I_[:, 0:T], TMP[:, 0:T], ALU.mult)),
                     reads=[ik, "WK6"], writes=[ik])
                if d_ == 0:
                    p.op(V, (lambda e, R=R, I_=I_, H=H: e.tensor_tensor_scan(
                        out=H[:, 0:T], data0=R[:, 0:T], data1=I_[:, 0:T], initial=0.0, op0=ALU.mult, op1=ALU.add)),
                        reads=[rk, ik], writes=[hk])
                else:
                    p.op(V, (lambda e, R=R, I_=I_, H=H: e.tensor_tensor_scan(
                        out=H[:, 0:T][:, ::-1], data0=R[:, 0:T][:, ::-1],
                        data1=I_[:, 0:T][:, ::-1], initial=0.0, op0=ALU.mult, op1=ALU.add)),
                        reads=[rk, ik], writes=[hk])
            GL = WKf[7]
            zchunk(4 + c, GL, "WK7", 0)
            p.op(A, lambda e: e.activation(out=GL[:, 0:T], in_=GL[:, 0:T], func=AF.Gelu_apprx_tanh),
                 reads=["WK7"], writes=["WK7"])
            H0, H1 = WKf[8], WKf[9]
            p.op(G, lambda e: e.tensor_tensor(H0[:, 0:T], H0[:, 0:T], H1[:, 0:T], ALU.add), reads=["WK8", "WK9"], writes=["WK8"])
            p.op(V, lambda e: e.tensor_tensor(H0[:, 0:T], H0[:, 0:T], GL[:, 0:T], ALU.mult), reads=["WK8", "WK7"], writes=["WK8"])
            p.op(A, (lambda e, c=c: e.copy(out=YB[:, c * T:(c + 1) * T], in_=H0[:, 0:T])), reads=["WK8"], writes=["YB"])
            p.op(G, lambda e: e.tensor_tensor(H1[:, 0:T], H0[:, 0:T], H0[:, 0:T], ALU.mult), reads=["WK8"], writes=["WK9"])
            sumsq_cols(H1, "WK9", c * 16)
            cv(12)

        p.op(G, lambda e: e.memset(WKb[3][:, 0:16], 0.0), writes=["WK3"])
        p.op(G, lambda e: e.memset(WKb[3][:, 14 + T:32 + T], 0.0), writes=["WK3"])
        for c in range(4):
            Aa, Bb, GLU, DG, CV, Dd, DSQ, RS = WKf[1], WKf[2], WKb[3], WKb[4], WKf[5], WKf[6], WKf[7], WKf[8]
            zchunk(8 + c, Aa, "WK1", 0)
            zchunk(12 + c, Bb, "WK2", 0)
            p.op(A, lambda e: e.activation(out=Bb[:, 0:T], in_=Bb[:, 0:T], func=AF.Sigmoid), reads=["WK2"], writes=["WK2"])
            p.op(V, lambda e: e.tensor_tensor(GLU[:, 15:15 + T], Aa[:, 0:T], Bb[:, 0:T], ALU.mult),
                 reads=["WK1", "WK2"], writes=["WK3"])
            for k in range(31):
                p.op(V, (lambda e, k=k, c=c: e.tensor_scalar(DG[:, k * 128:(k + 1) * 128], identb[:],
                                                             PP[:, 52 + k * 4 + c: 53 + k * 4 + c], 0.0, ALU.mult, ALU.add)),
                     reads=["identb", "PP"], writes=["WK4"])
            for tt in range(4):
                b = gbank()
                for k in range(31):
                    p.op(P_, (lambda e, b=b, k=k, tt=tt: e.matmul(bank(b), DG[:, k * 128:(k + 1) * 128],
                                                                  GLU[:, tt * 512 + k: tt * 512 + k + 512],
                                                                  start=(k == 0), stop=(k == 30))),
                         reads=["WK4", "WK3"], writes=[bk(b)])
                p.op(A, (lambda e, b=b, tt=tt, c=c: e.activation(out=CV[:, tt * 512:(tt + 1) * 512], in_=bank(b),
                                                                 func=AF.Identity, bias=PP[:, 176 + c:177 + c])),
                     reads=[bk(b), "PP"], writes=["WK5"])
            for tt in range(4):
                b = gbank()
                p.op(P_, (lambda e, b=b, tt=tt: e.matmul(bank(b), GM[:], CV[:, tt * 512:(tt + 1) * 512], start=True, stop=True)),
                     reads=["GM", "WK5"], writes=[bk(b)])
                p.op(V, (lambda e, b=b, tt=tt: e.tensor_tensor(Dd[:, tt * 512:(tt + 1) * 512], CV[:, tt * 512:(tt + 1) * 512],
                                                               bank(b), ALU.subtract)),
                     reads=[bk(b), "WK5"], writes=["WK6"])
            p.op(G, lambda e: e.tensor_tensor(DSQ[:, 0:T], Dd[:, 0:T], Dd[:, 0:T], ALU.mult), reads=["WK6"], writes=["WK7"])
            for tt in range(4):
                b = gbank()
                p.op(P_, (lambda e, b=b, tt=tt: e.matmul(bank(b), GM[:], DSQ[:, tt * 512:(tt + 1) * 512], start=True, stop=True)),
                     reads=["GM", "WK7"], writes=[bk(b)])
                p.op(A, (lambda e, b=b, tt=tt: e.activation(out=RS[:, tt * 512:(tt + 1) * 512], in_=bank(b), func=AF.Sqrt,
                                                            bias=EPSC, scale=1.0)),
                     reads=[bk(b), "SM"], writes=["WK8"])
            p.op(V, lambda e: e.reciprocal(RS[:, 0:T], RS[:, 0:T]), reads=["WK8"], writes=["WK8"])
            p.op(V, lambda e: e.tensor_tensor(Dd[:, 0:T], Dd[:, 0:T], RS[:, 0:T], ALU.mult), reads=["WK6", "WK8"], writes=["WK6"])
            p.op(A, (lambda e, c=c: e.activation(out=Dd[:, 0:T], in_=Dd[:, 0:T], func=AF.Silu,
                                                 bias=PP[:, 184 + c:185 + c], scale=PP[:, 180 + c:181 + c])),
                 reads=["WK6", "PP"], writes=["WK6"])
            p.op(A, (lambda e, c=c: e.copy(out=YB[:, (4 + c) * T:(5 + c) * T], in_=Dd[:, 0:T])), reads=["WK6"], writes=["YB"])
            p.op(G, lambda e: e.tensor_tensor(DSQ[:, 0:T], Dd[:, 0:T], Dd[:, 0:T], ALU.mult), reads=["WK6"], writes=["WK7"])
            sumsq_cols(DSQ, "WK7", 64 + c * 16)
            cv(12)

        p.barrier()
        p.op(V, lambda e: e.tensor_reduce(out=sub(RS2, 0, [[16, 2], [1, 16]]), in_=sub(bank(0), 0, [[64, 2], [1, 16], [16, 4]]), axis=AX.X, op=ALU.add),
             reads=[bk(0)], writes=["SMr"])
        p.op(A, lambda e: e.activation(out=RS2, in_=RS2, func=AF.Sqrt, bias=EPSC, scale=1.0 / 512), reads=["SMr", "SM"], writes=["SMr"])
        p.op(V, lambda e: e.reciprocal(RS2, RS2), reads=["SMr"], writes=["SMr"])
        for cc in range(8):
            stg = WK[2][:, (cc % 2) * 1024:(cc % 2) * 1024 + 1024]
            skey = "WK2_%d" % (cc % 2)
            wob = WKb[cc // 4][:, (cc % 4) * 1024:(cc % 4) * 1024 + 1024]
            p.dma("sync", (lambda e, cc=cc, stg=stg: e.dma_start(out=stg, in_=wout_d[cc * 128:(cc + 1) * 128, :])),
                  writes=[skey])
            p.op(V, (lambda e, cc=cc, stg=stg, wob=wob: e.tensor_scalar(wob, stg, PP[:, 188 + cc:189 + cc], 0.0, ALU.mult, ALU.add)),
                 reads=[skey, "PP"], writes=["WOB"])

        def h2tile(ti):
            if ti < 8:
                return XTBf[:, ti * 1024:(ti + 1) * 1024]
            return WINBf[:, (ti - 8) * 1024:(ti - 7) * 1024]

        for ti in range(16):
            XS = WK[3][:, (ti % 2) * 1024:(ti % 2) * 1024 + 1024]
            xkey = "XS%d" % (ti % 2)
            p.dma("sync", (lambda e, ti=ti, XS=XS: e.dma_start(out=XS, in_=x_d[ti * 128:(ti + 1) * 128, :])),
                  writes=[xkey])
            cv(1)
            H2t = h2tile(ti)
            hkey = "XTB" if ti < 8 else "WINB"
            for dh in range(2):
                bl, bc = 4 + dh, 6 + dh
                for c in range(4):
                    p.op(P_, (lambda e, bl=bl, c=c, ti=ti, dh=dh: e.matmul(
                        bank(bl), YB[:, c * T + ti * 128: c * T + ti * 128 + 128],
                        WKb[c // 4][:, (c % 4) * 1024 + dh * 512:(c % 4) * 1024 + dh * 512 + 512],
                        start=(c == 0), stop=(c == 3))), reads=["YB", "WOB"], writes=[bk(bl)])
                for c in range(4, 8):
                    p.op(P_, (lambda e, bc=bc, c=c, ti=ti, dh=dh: e.matmul(
                        bank(bc), YB[:, c * T + ti * 128: c * T + ti * 128 + 128],
                        WKb[c // 4][:, (c % 4) * 1024 + dh * 512:(c % 4) * 1024 + dh * 512 + 512],
                        start=(c == 4), stop=(c == 7))), reads=["YB", "WOB"], writes=[bk(bc)])
                p.op(V, (lambda e, bl=bl, ti=ti, dh=dh, XS=XS, H2t=H2t: e.scalar_tensor_tensor(
                    out=H2t[:, dh * 512:(dh + 1) * 512], in0=bank(bl), scalar=RS2[:, ti:ti + 1],
                    in1=XS[:, dh * 512:(dh + 1) * 512], op0=ALU.mult, op1=ALU.add)),
                    reads=[bk(bl), "SMr", xkey], writes=[hkey])
                p.op(V, (lambda e, bc=bc, ti=ti, dh=dh, H2t=H2t: e.scalar_tensor_tensor(
                    out=H2t[:, dh * 512:(dh + 1) * 512], in0=bank(bc), scalar=RS2[:, 16 + ti:17 + ti],
                    in1=H2t[:, dh * 512:(dh + 1) * 512], op0=ALU.mult, op1=ALU.add)),
                    reads=[bk(bc), "SMr", hkey], writes=[hkey])

        p.barrier()

        if stop_after == "A":
            for ti in range(16):
                p.dma("sync", (lambda e, ti=ti: e.dma_start(out=out_d[ti * 128:(ti + 1) * 128, :], in_=h2tile(ti))),
                      writes=["out%d" % ti])
            p.finish("sync", ["out%d" % ti for ti in range(16)])
            p.emit()
            return nc

        IDXT = YBu[:, 0:2048]
        SKT = YBf[:, 2048:4096]
        G2B = YBf[:, 4096:5120]
        GATET = YBf[:, 6144:8192]
        p.dma("sync", lambda e: e.dma_start(out=G2B, in_=g2_d.partition_broadcast(128)), writes=["G2B"])
        p.dma("sync", lambda e: e.dma_start(out=sub(SKT, 0, [[128, 16], [1, 128]]), in_=skT_d.rearrange("e c k -> c e k")),
              writes=["SKT"])
        WQB = [WKb[4 + dc // 2][:, (dc % 2) * 2048:(dc % 2) * 2048 + 2048] for dc in range(8)]
        for dc in range(8):
            stg = WK[8 + dc % 2][:, 0:2048]
            skey = "WQS%d" % (dc % 2)
            p.dma("sync", (lambda e, dc=dc, stg=stg: e.dma_start(out=stg, in_=wq_d[dc * 128:(dc + 1) * 128, :])), writes=[skey])
            p.op(V, (lambda e, dc=dc, stg=stg: e.tensor_copy(WQB[dc], stg)), reads=[skey], writes=["WQB"])
        for ti in range(16):
            p.op(A, (lambda e, ti=ti: e.activation(out=WK[1][:, 1024:2048], in_=h2tile(ti), func=AF.Square,
                                                   accum_out=SS2[:, ti:ti + 1])),
                 reads=[("h2", ti)], writes=["junkA", ("ss2", ti)])
        p.op(A, lambda e: e.activation(out=RSB, in_=SS2, func=AF.Sqrt, bias=EPSC, scale=1.0 / D),
             reads=[("ss2", ti) for ti in range(16)] + ["SM"], writes=["RSB"])
        p.op(V, lambda e: e.reciprocal(RSB, RSB), reads=["RSB"], writes=["RSB"])

        def reg(i, n=128):
            return WK[2][:, i * 128:i * 128 + n]

        TOP, TOPI, TOPIf = reg(0, 256), reg(2, 256).bitcast(U32), reg(4, 256)
        WRK, WRK2 = reg(6), reg(7, 256)
        TS, POS = reg(9), reg(10).bitcast(U32)
        PAu, PBu = reg(11).bitcast(U32), reg(12).bitcast(U32)
        PAf, PBf = reg(13), reg(14)
        E1, E2 = reg(15), WK[1][:, 0:128]
        EIDX, EX, GATE = WK[1][:, 128:256], WK[1][:, 256:384], WK[1][:, 384:512]
        Zs, RZ = WK[1][:, 512:520], WK[1][:, 520:528]
        C_ = WKf[0]
        OHt = RSTDb[:, 2048:4096]

        def n2tile(slot):
            return RSTDb[:, slot * 1024:(slot + 1) * 1024]

        def compute_n2(ti, slot):
            p.op(V, (lambda e, ti=ti, slot=slot: e.scalar_tensor_tensor(
                out=n2tile(slot), in0=h2tile(ti), scalar=RSB[:, ti:ti + 1], in1=G2B, op0=ALU.mult, op1=ALU.mult)),
                reads=[("h2", ti), "RSB", "G2B"], writes=[("n2", slot)])

        SBUFS = [[WK[8][:, 0:1024], WK[8][:, 1024:2048]], [WK[1][:, 1024:2048], YBf[:, 5120:6144]]]
        def b1_stage1(ti):
            cv(8)
            if lvl < 1:
                return
            slot = ti % 2
            compute_n2(ti, slot)
            N2 = n2tile(slot)
            PT = PS2[0][:, 0:512].bitcast(BF16)
            for dc in range(8):
                p.op(P_, (lambda e, dc=dc, N2=N2, PT=PT: e.transpose(PT[:, dc * 128:(dc + 1) * 128], N2[:, dc * 128:(dc + 1) * 128], identb[:])),
                     reads=[("n2", slot), "identb"], writes=[bk(0)])
            N2T = WKb[3][:, slot * 1024:(slot + 1) * 1024]
            p.op(A, (lambda e, N2T=N2T, PT=PT: e.copy(out=N2T, in_=PT)), reads=[bk(0)], writes=[("n2t", slot)])
            if lvl < 2:
                return
            QSB = WKf[9]
            Sh = SBUFS[ti % 2]
            for half in range(2):
                QP = PS2[1]
                for e8 in range(8):
                    ec = half * 8 + e8
                    for dc in range(8):
                        p.op(P_, (lambda e, e8=e8, ec=ec, dc=dc, N2T=N2T, QP=QP: e.matmul(
                            QP[:, e8 * 128:(e8 + 1) * 128], WQB[dc][:, ec * 128:(ec + 1) * 128],
                            N2T[:, dc * 128:(dc + 1) * 128], start=(dc == 0), stop=(dc == 7))),
                            reads=["WQB", ("n2t", slot)], writes=["QP"])
                p.op(A, (lambda e, half=half, QP=QP: e.copy(out=QSB[:, half * 1024:(half + 1) * 1024], in_=QP[:, :])),
                     reads=["QP"], writes=[("qsb", half)])
                SP = PS2[2 + half]
                for e8 in range(8):
                    ec = half * 8 + e8
                    p.op(P_, (lambda e, e8=e8, ec=ec, SP=SP: e.matmul(
                        SP[:, e8 * 128:(e8 + 1) * 128], QSB[:, ec * 128:(ec + 1) * 128], SKT[:, ec * 128:(ec + 1) * 128],
                        start=True, stop=True)), reads=[("qsb", half), "SKT"], writes=[("SP", half)])
                p.op(A, (lambda e, half=half, SP=SP, Sh=Sh: e.copy(out=Sh[half], in_=SP[:, :])),
                     reads=[("SP", half)], writes=[("S", ti % 2, half)])
        def b1_stage2(ti):
            Sh = SBUFS[ti % 2]
            if lvl < 3:
                return
            for g in range(16):
                Sg = Sh[g // 8][:, (g % 8) * 128:(g % 8) * 128 + 128]
                sk = ("S", ti % 2, g // 8)
                t0, t1 = TOP[:, g * 16:g * 16 + 8], TOP[:, g * 16 + 8:g * 16 + 16]
                i0, i1 = TOPI[:, g * 16:g * 16 + 8], TOPI[:, g * 16 + 8:g * 16 + 16]
                p.op(V, (lambda e, Sg=Sg, t0=t0: e.max(out=t0, in_=Sg)), reads=[sk], writes=["TOP"])
                p.op(V, (lambda e, Sg=Sg, t0=t0, i0=i0: e.max_index(out=i0, in_max=t0, in_values=Sg)), reads=[sk, "TOP"], writes=["TOPI"])
                p.op(V, (lambda e, Sg=Sg, t0=t0: e.match_replace(out=WRK, in_to_replace=t0, in_values=Sg, imm_value=-1e30)),
                     reads=[sk, "TOP"], writes=["WRK"])
                p.op(V, (lambda e, t1=t1: e.max(out=t1, in_=WRK)), reads=["WRK"], writes=["TOP"])
                p.op(V, (lambda e, t1=t1, i1=i1: e.max_index(out=i1, in_max=t1, in_values=WRK)), reads=["WRK", "TOP"], writes=["TOPI"])
            p.op(V, lambda e: e.tensor_copy(TOPIf, TOPI), reads=["TOPI"], writes=["TOPIf"])
            if lvl < 4:
                return
            p.op(V, lambda e: e.tensor_tensor(sub(C_, 0, [[256, 8], [16, 16], [1, 16]]),
                                              sub(TOP, 0, [[32, 8], [1, 16], [0, 16]]),
                                              sub(TOP, 16, [[32, 8], [0, 16], [1, 16]]), ALU.add),
                 reads=["TOP"], writes=["C"])
            for h in range(8):
                Ch = C_[:, h * 256:(h + 1) * 256]
                t0, t1 = TS[:, h * 16:h * 16 + 8], TS[:, h * 16 + 8:h * 16 + 16]
                i0, i1 = POS[:, h * 16:h * 16 + 8], POS[:, h * 16 + 8:h * 16 + 16]
                p.op(V, (lambda e, Ch=Ch, t0=t0: e.max(out=t0, in_=Ch)), reads=["C"], writes=["TS"])
                p.op(V, (lambda e, Ch=Ch, t0=t0, i0=i0: e.max_index(out=i0, in_max=t0, in_values=Ch)), reads=["C", "TS"], writes=["POS"])
                p.op(V, (lambda e, Ch=Ch, t0=t0: e.match_replace(out=WRK2, in_to_replace=t0, in_values=Ch, imm_value=-1e30)),
                     reads=["C", "TS"], writes=["WRK2"])
                p.op(V, (lambda e, t1=t1: e.max(out=t1, in_=WRK2)), reads=["WRK2"], writes=["TS"])
                p.op(V, (lambda e, t1=t1, i1=i1: e.max_index(out=i1, in_max=t1, in_values=WRK2)), reads=["WRK2", "TS"], writes=["POS"])
            if lvl < 5:
                return
            p.op(V, lambda e: e.tensor_single_scalar(PAu, POS, 4, ALU.logical_shift_right), reads=["POS"], writes=["PAu"])
            p.op(V, lambda e: e.tensor_single_scalar(PBu, POS, 15, ALU.bitwise_and), reads=["POS"], writes=["PBu"])
            p.op(V, lambda e: e.tensor_copy(PAf, PAu), reads=["PAu"], writes=["PAf"])
            p.op(V, lambda e: e.tensor_copy(PBf, PBu), reads=["PBu"], writes=["PBf"])
            for (PXf, pk, off, Eo, ek) in ((PAf, "PAf", 0, E1, "E1"), (PBf, "PBf", 16, E2, "E2")):
                p.op(V, (lambda e, PXf=PXf: e.tensor_tensor(sub(OHt, 0, [[256, 8], [16, 16], [1, 16]]),
                                                            sub(IOTA, 0, [[0, 8], [0, 16], [1, 16]]),
                                                            sub(PXf, 0, [[16, 8], [1, 16], [0, 16]]), ALU.is_equal)),
                     reads=[pk, "SM"], writes=["OH"])
                p.op(V, (lambda e, off=off: e.tensor_tensor(sub(OHt, 0, [[256, 8], [16, 16], [1, 16]]),
                                                            sub(OHt, 0, [[256, 8], [16, 16], [1, 16]]),
                                                            sub(TOPIf, off, [[32, 8], [0, 16], [1, 16]]), ALU.mult)),
                     reads=["OH", "TOPIf"], writes=["OH"])
                p.op(V, (lambda e, Eo=Eo: e.tensor_reduce(out=Eo, in_=sub(OHt, 0, [[16, 128], [1, 16]]), axis=AX.X, op=ALU.add)),
                     reads=["OH"], writes=[ek])
            p.op(V, lambda e: e.scalar_tensor_tensor(out=EIDX, in0=E1, scalar=128.0, in1=E2, op0=ALU.mult, op1=ALU.add),
                 reads=["E1", "E2"], writes=["EIDX"])
            if lvl < 6:
                return
            p.op(V, lambda e: e.tensor_tensor(sub(EX, 0, [[16, 8], [1, 16]]), sub(TS, 0, [[16, 8], [1, 16]]),
                                              sub(TS, 0, [[16, 8], [0, 16]]), ALU.subtract), reads=["TS"], writes=["EX"])
            p.op(A, lambda e: e.activation(out=EX, in_=EX, func=AF.Exp), reads=["EX"], writes=["EX"])
            p.op(V, lambda e: e.tensor_reduce(out=Zs, in_=sub(EX, 0, [[16, 8], [1, 16]]), axis=AX.X, op=ALU.add), reads=["EX"], writes=["Zs"])
            p.op(V, lambda e: e.reciprocal(RZ, Zs), reads=["Zs"], writes=["RZ"])
            p.op(V, lambda e: e.tensor_tensor(sub(GATE, 0, [[16, 8], [1, 16]]), sub(EX, 0, [[16, 8], [1, 16]]),
                                              sub(RZ, 0, [[1, 8], [0, 16]]), ALU.mult), reads=["EX", "RZ"], writes=["GATE"])
            if lvl < 7:
                return
            p.op(P_, lambda e: e.matmul(PS2[0][:, 512:640], EIDX, ident[:], start=True, stop=True), reads=["EIDX", "ident"], writes=[bk(1)])
            p.op(P_, lambda e: e.matmul(PS2[0][:, 640:768], GATE, ident[:], start=True, stop=True), reads=["GATE", "ident"], writes=[bk(1)])
            p.op(V, (lambda e, ti=ti: e.tensor_copy(IDXT[:, ti * 128:(ti + 1) * 128], PS2[0][:, 512:640])), reads=[bk(1)], writes=[("idxt", ti)])
            p.op(A, (lambda e, ti=ti: e.copy(out=GATET[:, ti * 128:(ti + 1) * 128], in_=PS2[0][:, 640:768])), reads=[bk(1)], writes=[("gatet", ti)])

        b1_stage1(0)
        for ti in range(16):
            if ti + 1 < 16:
                b1_stage1(ti + 1)
            b1_stage2(ti)
        cv(NPIECE)
        p.barrier()
        if stop_after == "B1":
            for q_, (c0) in enumerate((0, 1024, 6144, 7168)):
                p.dma("sync", (lambda e, q_=q_, c0=c0: e.dma_start(out=out_d[q_ * 128:(q_ + 1) * 128, :], in_=YBf[:, c0:c0 + 1024])),
                      writes=["out%d" % q_])
            p.finish("sync", ["out%d" % q_ for q_ in range(4)])
            p.emit()
            return nc

        GFB = WK[9][:, 1024:2048]
        OUTT = WK[9][:, 0:1024]
        ZC = WKb[8][:, 0:256]
        ACTM = WK[8][:, 256:384]
        GA = WK[8][:, 384:512]
        LTG = [WKb[8][:, 1024:2048], WKb[8][:, 2048:3072]]
        JUNK = WKb[8][:, 3072:4096]
        JUNK2 = WKb[8][:, 3072:4096]
        p.dma("sync", lambda e: e.dma_start(out=GFB, in_=gf_d.partition_broadcast(128)), writes=["GFB"])
        p.op(V, lambda e: e.memset(ZC, 0.0), writes=["ZC"])
        p.op(V, lambda e: e.memset(ZC[:, 128:129], 1.0), reads=["ZC"], writes=["ZC"])
        NS = 16
        GS = 8
        SL = [WKb[s_ // 2][:, (s_ % 2) * 2048:(s_ % 2) * 2048 + 2048] for s_ in range(16)]
        CVIb = CVI[:].bitcast(BF16)
        SL += [CVIb[:, 0:2048], CVIb[:, 2048:4096], CVO[:, 0:2048]]
        NS = len(SL)
        LAG = 13
        XSB = [RSTDb[:, 2048:3072], RSTDb[:, 3072:4096]]
        PRB = [YB[:, 10240:11264], YB[:, 11264:12288]]
        NTOK = 16 * 128
        for step in range(NTOK + LAG):
            nu, nv = step, step - LAG
            if nu < NTOK:
                ti, t, s_ = nu // 128, nu % 128, nu % NS
                if t == 0:
                    compute_n2(ti, ti % 2)
                N2 = n2tile(ti % 2)
                XBP = PS2[nu % 3]
                xk = ("xbp", nu % 3)
                p.dma(G, (lambda e, s_=s_, nu=nu: e.indirect_dma_start(
                    out=SL[s_], out_offset=None, in_=puv_d,
                    in_offset=bass.IndirectOffsetOnAxis(ap=IDXT[:, nu:nu + 1], axis=0))),
                    reads=[("idxt", ti)], writes=[("sl", s_)], stream=("g", s_))
                for half in range(2):
                    p.op(P_, (lambda e, t=t, half=half, XBP=XBP, N2=N2: e.matmul(
                        XBP[:, half * 512:(half + 1) * 512], identb[:, t:t + 1].to_broadcast([128, 128]),
                        N2[:, half * 512:(half + 1) * 512], start=True, stop=True)),
                        reads=["identb", ("n2", ti % 2)], writes=[xk])
            mm = step - 4
            if 0 <= mm < NTOK and mm % GS == GS - 1:
                ti_m, g0 = mm // 128, (mm % 128) - (GS - 1)
                gk_m = ("ga", (mm // GS) % 16)
                p.op(V, (lambda e, g0=g0, ti_m=ti_m: e.tensor_tensor(GA[:, g0:g0 + GS], GA[:, g0:g0 + GS],
                                                                     GATET[:, ti_m * 128 + g0: ti_m * 128 + g0 + GS], ALU.mult)),
                     reads=[gk_m, ("gatet", ti_m)], writes=[gk_m])
                gsl = (mm // GS) % 2
                p.op(V, (lambda e, g0=g0, gsl=gsl: e.tensor_tensor(
                    sub(LTG[gsl], 0, [[128, GS], [1, 128]]),
                    sub(ZC, 128 - g0, [[-1, GS], [1, 128]]),
                    sub(GA, g0, [[1, GS], [0, 128]]), ALU.mult)),
                    reads=["ZC", gk_m], writes=[("ltg", gsl)])
            if nv >= 0:
                ti, t, s_ = nv // 128, nv % 128, nv % NS
                OP = PS2[3]
                ok = ("op", 0)
                gsl_v = (nv // GS) % 2
                lt = LTG[gsl_v][:, (nv % GS) * 128:(nv % GS) * 128 + 128]
                for half in range(2):
                    p.op(P_, (lambda e, s_=s_, t=t, half=half, lt=lt, OP=OP: e.matmul(
                        OP[:, half * 512:(half + 1) * 512], lt, SL[s_][:, 1024 + half * 512:1024 + (half + 1) * 512],
                        start=(t == 0), stop=(t == 127))),
                        reads=[("ltg", gsl_v), ("sl", s_)], writes=[ok])
            if nu < NTOK:
                ti, t, s_ = nu // 128, nu % 128, nu % NS
                if nu % 4 != 3:
                    p.op(V, (lambda e, s_=s_, t=t, XBP=XBP: e.scalar_tensor_tensor(
                        out=JUNK, in0=SL[s_][:, 0:1024], scalar=1.0, in1=XBP[:, :], op0=ALU.mult, op1=ALU.mult,
                        accum_out=ACTM[:, t:t + 1])),
                        reads=[("sl", s_), xk], writes=[("actm", t)])
                else:
                    q_ = (nu // 4) % 2
                    p.op(A, (lambda e, q_=q_, XBP=XBP: e.copy(out=XSB[q_], in_=XBP[:, :])), reads=[xk], writes=[("xsb", q_)])
                    p.op(V, (lambda e, q_=q_, s_=s_: e.tensor_tensor(PRB[q_], SL[s_][:, 0:1024], XSB[q_], ALU.mult)),
                         reads=[("sl", s_), ("xsb", q_)], writes=[("prb", q_)])
            ao = step - 2
            if 0 <= ao < NTOK and ao % 4 == 3:
                q_ = (ao // 4) % 2
                t_ao = ao % 128
                p.op(A, (lambda e, q_=q_, t_ao=t_ao: e.activation(out=JUNK2, in_=PRB[q_], func=AF.Identity,
                                                                  accum_out=ACTM[:, t_ao:t_ao + 1])),
                     reads=[("prb", q_)], writes=[("actm", t_ao)])
                if t_ao % GS == GS - 1:
                    g0 = t_ao - (GS - 1)
                    gk = ("ga", (ao // GS) % 16)
                    p.op(A, (lambda e, g0=g0: e.activation(out=GA[:, g0:g0 + GS], in_=ACTM[:, g0:g0 + GS], func=AF.Gelu_apprx_tanh)),
                         reads=[("actm", tt_) for tt_ in range(g0, g0 + GS)], writes=[gk])
            if nv >= 0 and nv % 128 == 127:
                ti = nv // 128
                OP = PS2[3]
                ok = ("op", 0)
                p.op(V, (lambda e, ti=ti, OP=OP: e.tensor_tensor(OUTT, h2tile(ti), OP[:, :], ALU.add)),
                     reads=[("h2", ti), ok], writes=["OUTT"])
                p.op(A, lambda e: e.activation(out=JUNK2, in_=OUTT, func=AF.Square, accum_out=SS3), reads=["OUTT"], writes=["JUNK2", "SS3"])
                p.op(A, lambda e: e.activation(out=R3, in_=SS3, func=AF.Sqrt, bias=EPSC, scale=1.0 / D), reads=["SS3", "SM"], writes=["R3"])
                p.op(V, lambda e: e.reciprocal(R3, R3), reads=["R3"], writes=["R3"])
                p.op(V, lambda e: e.scalar_tensor_tensor(out=OUTT, in0=OUTT, scalar=R3, in1=GFB, op0=ALU.mult, op1=ALU.mult),
                     reads=["OUTT", "R3", "GFB"], writes=["OUTT"])
                p.dma("sync", (lambda e, ti=ti: e.dma_start(out=out_d[ti * 128:(ti + 1) * 128, :], in_=OUTT)),
                      reads=["OUTT"], writes=["out%d" % ti])
        p.finish("sync", ["out%d" % ti for ti in range(16)])
        p.emit()
    return nc


def make_in_maps(x, mix_norm_g, w_in, lru_conv_w, lru_conv_b, lru_w_rg, lru_b_rg, lru_w_ig, lru_b_ig,
                 lru_lambda, conf_conv_w, conf_conv_b, conf_norm_g, conf_norm_b, beta_lru, beta_conv,
                 w_out, ffn_norm_g, peer_w_q, peer_sub_keys, peer_u, peer_v, final_norm_g):
    f = lambda a: np.ascontiguousarray(np.asarray(a, dtype=np.float32))

    def cols(v):
        return f(v).reshape(-1, 128).T

    pp = np.concatenate([
        cols(mix_norm_g[0]),
        np.concatenate([cols(lru_conv_w[0][k]) for k in range(4)], axis=1),
        cols(lru_conv_b[0]),
        cols(lru_b_rg[0][0]), cols(lru_b_ig[0][0]), cols(lru_b_rg[0][1]), cols(lru_b_ig[0][1]),
        cols(lru_lambda[0][0]), cols(lru_lambda[0][1]),
        np.concatenate([cols(conf_conv_w[0][k]) for k in range(31)], axis=1),
        cols(conf_conv_b[0]), cols(conf_norm_g[0]), cols(conf_norm_b[0]),
        cols(beta_lru[0]), cols(beta_conv[0]),
    ], axis=1)
    pp = f(pp)
    assert pp.shape == (128, NPP)
    wg = f(np.stack([lru_w_rg[0][0], lru_w_ig[0][0], lru_w_rg[0][1], lru_w_ig[0][1]], axis=0))
    skT = f(np.transpose(np.asarray(peer_sub_keys[0]), (0, 1, 3, 2)).reshape(16, 128, 128))
    shared = {
        "w_in": f(w_in[0]), "w_out": f(w_out[0]), "w_q": f(peer_w_q[0]), "skT": skT,
        "pu": f(peer_u[0]), "pv": f(peer_v[0]), "pp": pp, "wg": wg,
        "g2": f(ffn_norm_g[0]).reshape(1, D), "gf": f(final_norm_g).reshape(1, D),
    }
    xs = np.asarray(x, dtype=np.float32)
    maps = []
    for b in range(xs.shape[0]):
        m = dict(shared)
        m["x"] = f(xs[b])
        m["xT"] = f(xs[b].T)
        maps.append(m)
    return maps


_NC = None


def kernel(**inputs):
    global _NC
    in_maps = make_in_maps(**inputs)
    if _NC is None:
        _NC = build()
    res = run_bass_kernel_spmd(_NC, in_maps, core_ids=list(range(len(in_maps))))
    return np.stack([np.asarray(r["out"], dtype=np.float32) for r in res.results], axis=0)
```

```python
import numpy as np
from contextlib import ExitStack
import concourse.bass as bass
import concourse.mybir as mybir
from concourse.bass_utils import run_bass_kernel_spmd

F32 = mybir.dt.float32
BF16 = mybir.dt.bfloat16
U32 = mybir.dt.uint32
I32 = mybir.dt.int32
AF = mybir.ActivationFunctionType
ALU = mybir.AluOpType
AX = mybir.AxisListType

T = 2048
D = 1024
EPS = 1e-6
NPP = 196
WKC = 2056


class Prog:
    def __init__(self, nc):
        self.nc = nc
        self.ops = {e: [] for e in ("sync", "scalar", "vector", "gpsimd", "tensor")}
        self.cnt = {}
        self.waited = {e: {} for e in self.ops}
        self.last_w = {}
        self.readers = {}
        self.semkeys = []

    def _semkey(self, k):
        if k not in self.cnt:
            self.cnt[k] = 0
            self.semkeys.append(k)
        return k

    def _deps(self, eng, reads, writes, own_key, skip_same=False):
        need = {}

        def add(k, v):
            if skip_same and k == own_key:
                return
            if need.get(k, 0) < v:
                need[k] = v

        for b in reads:
            d = self.last_w.get(b)
            if d is not None:
                add(*d)
        for b in writes:
            d = self.last_w.get(b)
            if d is not None:
                add(*d)
            for k, v in self.readers.get(b, {}).items():
                add(k, v)
        waits = []
        for k, v in need.items():
            if self.waited[eng].get(k, 0) < v:
                self.waited[eng][k] = v
                waits.append((k, v))
        return waits

    def _commit(self, reads, writes, key, val):
        for b in reads:
            r = self.readers.setdefault(b, {})
            if r.get(key, 0) < val:
                r[key] = val
        for b in writes:
            self.last_w[b] = (key, val)
            self.readers[b] = {}

    @staticmethod
    def _ispsum(k):
        return k == "QP" or (isinstance(k, tuple) and k[0] in ("pb", "SP", "xbp", "op"))

    def op(self, eng, fn, reads=(), writes=()):
        ps = [r for r in reads if self._ispsum(r)]
        if ps:
            reads = [r for r in reads if not self._ispsum(r)]
            writes = list(writes) + ps
        key = self._semkey(eng)
        waits = self._deps(eng, reads, writes, key, skip_same=(eng == "tensor"))
        self.cnt[key] += 1
        self.ops[eng].append((waits, fn, key, 1))
        self._commit(reads, writes, key, self.cnt[key])

    def dma(self, queue, fn, reads=(), writes=(), stream=None):
        if stream is None:
            stream = tuple(writes)
        key = self._semkey(("dma", stream))
        waits = self._deps(queue, reads, writes, key)
        self.cnt[key] += 16
        self.ops[queue].append((waits, fn, key, 16))
        self._commit(reads, writes, key, self.cnt[key])

    def barrier(self):
        for e in self.ops:
            waits = []
            for k in self.semkeys:
                v = self.cnt[k]
                if v > 0 and self.waited[e].get(k, 0) < v:
                    self.waited[e][k] = v
                    waits.append((k, v))
            self.ops[e].append((waits, None, None, 0))
        self.last_w = {}
        self.readers = {}

    def finish(self, eng, bufs):
        waits = self._deps(eng, bufs, (), None)
        self.ops[eng].append((waits, None, None, 0))

    def emit(self):
        nc = self.nc
        with ExitStack() as st:
            sems = {}
            for i, k in enumerate(self.semkeys):
                sems[k] = st.enter_context(nc.semaphore("s%d" % i))
            block = st.enter_context(nc.Block())

            def run(engname):
                def body(e):
                    for waits, fn, key, inc in self.ops[engname]:
                        for k, v in waits:
                            e.wait_ge(sems[k], v)
                        if fn is not None:
                            fn(e).then_inc(sems[key], inc)
                return body

            block.sync(run("sync"))
            block.scalar(run("scalar"))
            block.vector(run("vector"))
            block.gpsimd(run("gpsimd"))
            block.tensor(run("tensor"))


def sub(ap, off, dims):
    return bass.AP(ap.tensor, ap.offset + off, [list(ap.ap[0])] + [list(d) for d in dims])


def build(stop_after=None, lvl=99):
    nc = bass.Bass("TRN2", target_bir_lowering=False)

    def dram(name, shape, dt=F32, kind="ExternalInput"):
        return nc.dram_tensor(name, shape, dt, kind=kind).ap()

    xT_d = dram("xT", [D, T])
    x_d = dram("x", [T, D])
    win_d = dram("w_in", [D, 2048])
    wout_d = dram("w_out", [D, D])
    wq_d = dram("w_q", [D, 2048])
    skT_d = dram("skT", [16, 128, 128])
    pu_d = dram("pu", [16384, D])
    pv_d = dram("pv", [16384, D])
    pp_d = dram("pp", [128, NPP])
    wg_d = dram("wg", [4, 8, 64, 64])
    g2_d = dram("g2", [1, D])
    gf_d = dram("gf", [1, D])
    out_d = dram("out", [T, D], kind="ExternalOutput")
    puv_d = dram("puv", [16384, 2 * D], BF16, kind="Internal")

    with ExitStack() as st:
        def sb(name, shape, dt):
            return st.enter_context(nc.sbuf_tensor(name, shape, dt))

        WK = [sb("wk%d" % i, [128, WKC], F32) for i in range(10)]
        XTB = sb("xtb", [128, 8 * T], BF16)
        WINB = sb("winb", [128, 8 * 2048], BF16)
        YB = sb("yb", [128, 8 * T], BF16)
        RSTD = sb("rstd", [128, T], F32)
        PP = sb("pp_sb", [128, NPP], F32)
        BDB = sb("bdb", [128, 2048], BF16)
        ident = sb("ident", [128, 128], F32)
        identb = sb("identb", [128, 128], BF16)
        ones = sb("ones", [128, 128], F32)
        GM = sb("gm", [128, 128], F32)
        SM = sb("sm", [128, 256], F32)
        CVI = sb("cvi", [128, 2048], F32)
        CVO = sb("cvo", [128, 2048], BF16)
        PS2 = [st.enter_context(nc.psum_tensor("ps%d" % i, [128, 1024], F32)) for i in range(4)]

        WKf = [w[:] for w in WK]
        WKb = [w[:].bitcast(BF16) for w in WK]
        XTBf = XTB[:].bitcast(F32)
        WINBf = WINB[:].bitcast(F32)
        YBf = YB[:].bitcast(F32)
        YBu = YB[:].bitcast(U32)
        RSTDb = RSTD[:].bitcast(BF16)

        def bank(b):
            return PS2[b // 2][:, (b % 2) * 512:(b % 2) * 512 + 512]

        def bk(b):
            return ("pb", b)

        CL = SM[:, 0:8]
        CL2 = SM[:, 8:16]
        EPSC = SM[:, 16:17]
        ONEC = SM[:, 17:18]
        LSG = SM[:, 18:26]
        RS2 = SM[:, 32:64]
        SS2 = SM[:, 64:80]
        RSB = SM[:, 80:96]
        SS3 = SM[:, 96:97]
        R3 = SM[:, 97:98]
        IOTA = SM[:, 128:144]
        IOTAi = SM[:, 144:160].bitcast(I32)

        p = Prog(nc)
        V, A, G, P_ = "vector", "scalar", "gpsimd", "tensor"

        cv_src = [pu_d.rearrange("(p r) d -> p (r d)", p=128), pv_d.rearrange("(p r) d -> p (r d)", p=128)]
        puv_v = puv_d.rearrange("(p r) d -> p r d", p=128)
        NPIECE = 256
        cvn = [0]

        def cv_in(i):
            if i >= NPIECE:
                return
            tb, j, sl = i // 128, i % 128, i % 2
            p.dma(G, (lambda e, tb=tb, j=j, sl=sl: e.dma_start(out=CVI[:, sl * 1024:(sl + 1) * 1024],
                                                             in_=cv_src[tb][:, j * 1024:(j + 1) * 1024])),
                  writes=[("cvi", sl)], stream=("cvi", sl))

        def cv(n):
            for _ in range(n):
                i = cvn[0]
                if i >= NPIECE:
                    return
                if i == 0:
                    cv_in(0)
                    cv_in(1)
                tb, j, sl = i // 128, i % 128, i % 2
                p.op(G, (lambda e, sl=sl: e.tensor_copy(CVO[:, sl * 1024:(sl + 1) * 1024], CVI[:, sl * 1024:(sl + 1) * 1024])),
                     reads=[("cvi", sl)], writes=[("cvo", sl)])
                p.dma(G, (lambda e, tb=tb, j=j, sl=sl: e.dma_start(out=puv_v[:, j, tb * 1024:(tb + 1) * 1024],
                                                                 in_=CVO[:, sl * 1024:(sl + 1) * 1024])),
                      reads=[("cvo", sl)], writes=[("cvd", i)], stream=("cvo", sl))
                cv_in(i + 2)
                cvn[0] += 1

        p.dma("sync", lambda e: e.dma_start(out=PP[:], in_=pp_d), writes=["PP"])
        p.op(G, lambda e: e.memset(ident[:], 0.0), writes=["ident"])
        p.op(G, lambda e: e.affine_select(out=ident[:], in_=ident[:], pattern=[[-1, 128]],
                                          compare_op=ALU.not_equal, fill=1.0, base=0,
                                          channel_multiplier=1), reads=["ident"], writes=["ident"])
        p.op(V, lambda e: e.tensor_copy(identb[:], ident[:]), reads=["ident"], writes=["identb"])
        p.op(G, lambda e: e.memset(ones[:], 1.0), writes=["ones"])
        p.op(G, lambda e: e.memset(GM[:], 0.0), writes=["GM"])
        p.op(G, lambda e: e.memset(GM[0:64, 0:64], 1.0 / 64), reads=["GM"], writes=["GM"])
        p.op(G, lambda e: e.memset(GM[64:128, 64:128], 1.0 / 64), reads=["GM"], writes=["GM"])
        p.op(G, lambda e: e.memset(SM[:], 0.0), writes=["SM"])
        p.op(G, lambda e: e.memset(EPSC, EPS), reads=["SM"], writes=["SM"])
        p.op(G, lambda e: e.memset(ONEC, 1.0), reads=["SM"], writes=["SM"])
        p.op(G, lambda e: e.iota(IOTAi, pattern=[[1, 16]], base=0, channel_multiplier=0),
             reads=["SM"], writes=["SM"])
        p.op(V, lambda e: e.tensor_copy(IOTA, IOTAi), reads=["SM"], writes=["SM"])
        p.op(G, lambda e: e.memset(WKf[0], 0.0), writes=["WK0"])
        p.op(G, lambda e: e.memset(WKf[3], 0.0), writes=["WK3"])
        p.op(G, lambda e: e.memset(WKf[9], 0.0), writes=["WK9"])
        for kind in range(4):
            for c in range(4):
                for hl in range(2):
                    col = (kind * 4 + c) * 128 + hl * 64
                    p.dma("sync", (lambda e, kind=kind, c=c, hl=hl, col=col: e.dma_start(
                        out=WK[9][hl * 64:(hl + 1) * 64, col:col + 64], in_=wg_d[kind, 2 * c + hl])),
                        writes=["WK9"])
        p.op(A, lambda e: e.copy(out=BDB[:], in_=WK[9][:, 0:2048]), reads=["WK9"], writes=["BDB"])
        p.op(A, lambda e: e.activation(out=LSG, in_=PP[:, 44:52], func=AF.Sigmoid), reads=["PP", "SM"], writes=["SM"])
        p.op(A, lambda e: e.activation(out=LSG, in_=LSG, func=AF.Ln), reads=["SM"], writes=["SM"])
        p.op(V, lambda e: e.tensor_scalar(CL, LSG, 8.0, None, ALU.mult), reads=["SM"], writes=["SM"])
        p.op(V, lambda e: e.tensor_scalar(CL2, LSG, 16.0, None, ALU.mult), reads=["SM"], writes=["SM"])

        for c in range(8):
            stg = 1 + c % 2
            sq = 4 + c % 2
            p.dma("sync", (lambda e, c=c, stg=stg: e.dma_start(out=WK[stg][:, 0:T], in_=xT_d[c * 128:(c + 1) * 128, :])),
                  writes=["WK%d" % stg])
            p.op(A, (lambda e, stg=stg, sq=sq: e.activation(out=WK[sq][:, 0:T], in_=WK[stg][:, 0:T], func=AF.Square)),
                 reads=["WK%d" % stg], writes=["WK%d" % sq])
            p.op(V, (lambda e, c=c, stg=stg: e.tensor_copy(XTB[:, c * T:(c + 1) * T], WK[stg][:, 0:T])),
                 reads=["WK%d" % stg], writes=["XTB"])
            for tt in range(4):
                p.op(P_, (lambda e, c=c, sq=sq, tt=tt: e.matmul(bank(tt), ones[:], WK[sq][:, tt * 512:(tt + 1) * 512],
                                                                 start=(c == 0), stop=(c == 7))),
                     reads=["ones", "WK%d" % sq], writes=[bk(tt)])
        cv(16)
        for tt in range(4):
            p.op(A, (lambda e, tt=tt: e.activation(out=RSTD[:, tt * 512:(tt + 1) * 512], in_=bank(tt), func=AF.Sqrt,
                                                   bias=EPSC, scale=1.0 / D)),
                 reads=[bk(tt), "SM"], writes=["RSTD"])
        p.op(V, lambda e: e.reciprocal(RSTD[:], RSTD[:]), reads=["RSTD"], writes=["RSTD"])
        for c in range(8):
            stg = 6 + c % 2
            p.dma("sync", (lambda e, c=c, stg=stg: e.dma_start(out=WK[stg][:, 0:2048], in_=win_d[c * 128:(c + 1) * 128, :])),
                  writes=["WK%d" % stg])
            p.op(V, (lambda e, c=c, stg=stg: e.tensor_scalar(WINB[:, c * 2048:(c + 1) * 2048], WK[stg][:, 0:2048],
                                                             PP[:, c:c + 1], 0.0, ALU.mult, ALU.add)),
                 reads=["WK%d" % stg, "PP"], writes=["WINB"])

        zcnt = [0]

        def zchunk(f, dst, dkey, doff):
            for tt in range(4):
                b = 4 + zcnt[0] % 2
                zcnt[0] += 1
                for dc in range(8):
                    p.op(P_, (lambda e, b=b, dc=dc, tt=tt, f=f: e.matmul(
                        bank(b), WINB[:, dc * 2048 + f * 128: dc * 2048 + f * 128 + 128],
                        XTB[:, dc * T + tt * 512: dc * T + tt * 512 + 512], start=(dc == 0), stop=(dc == 7))),
                        reads=["WINB", "XTB"], writes=[bk(b)])
                p.op(V, (lambda e, b=b, tt=tt, dst=dst, doff=doff: e.tensor_tensor(
                    dst[:, doff + tt * 512: doff + tt * 512 + 512], bank(b), RSTD[:, tt * 512:(tt + 1) * 512], ALU.mult)),
                    reads=[bk(b), "RSTD"], writes=[dkey])

        gcnt = [0]

        def gbank():
            b = 6 + gcnt[0] % 2
            gcnt[0] += 1
            return b

        def sumsq_cols(src, skey, colbase):
            for ti in range(16):
                p.op(P_, (lambda e, ti=ti, src=src, colbase=colbase: e.matmul(
                    bank(0)[:, colbase + ti: colbase + ti + 1], src[:, ti * 128:(ti + 1) * 128], ones[:, 0:1],
                    start=True, stop=True)), reads=[skey, "ones"], writes=[bk(0)])

        for c in range(4):
            XL, XC, TMP, XCBt = WKf[0], WKf[1], WKf[6], WKb[7]
            zchunk(c, XL, "WK0", 2)
            p.op(V, (lambda e, c=c: e.tensor_scalar(XC[:, 0:T], XL[:, 0:T], PP[:, 8 + c:9 + c], PP[:, 24 + c:25 + c],
                                                    ALU.mult, ALU.add)), reads=["WK0", "PP"], writes=["WK1"])
            for k in range(1, 4):
                p.op(V, (lambda e, c=c, k=k: e.scalar_tensor_tensor(
                    out=XC[:, 0:T], in0=XL[:, k:k + T], scalar=PP[:, 8 + k * 4 + c: 9 + k * 4 + c], in1=XC[:, 0:T],
                    op0=ALU.mult, op1=ALU.add)), reads=["WK0", "WK1", "PP"], writes=["WK1"])
            p.op(A, lambda e: e.copy(out=XCBt[:, 0:T], in_=XC[:, 0:T]), reads=["WK1"], writes=["WK7"])
            for kind in range(4):
                Gt = WKf[2 + kind]
                for tt in range(4):
                    b = gbank()
                    p.op(P_, (lambda e, b=b, kind=kind, c=c, tt=tt: e.matmul(
                        bank(b), BDB[:, (kind * 4 + c) * 128:(kind * 4 + c) * 128 + 128],
                        XCBt[:, tt * 512:(tt + 1) * 512], start=True, stop=True)),
                        reads=["BDB", "WK7"], writes=[bk(b)])
                    p.op(A, (lambda e, b=b, kind=kind, c=c, tt=tt, Gt=Gt: e.activation(
                        out=Gt[:, tt * 512:(tt + 1) * 512], in_=bank(b), func=AF.Sigmoid,
                        bias=PP[:, 28 + kind * 4 + c: 29 + kind * 4 + c])),
                        reads=[bk(b), "PP"], writes=["WK%d" % (2 + kind)])
            for d_ in range(2):
                R, I_, H = WKf[2 + 2 * d_], WKf[3 + 2 * d_], WKf[8 + d_]
                rk, ik, hk = "WK%d" % (2 + 2 * d_), "WK%d" % (3 + 2 * d_), "WK%d" % (8 + d_)
                col = d_ * 4 + c
                p.op(A, (lambda e, R=R, col=col: e.activation(out=TMP[:, 0:T], in_=R[:, 0:T], func=AF.Exp,
                                                               scale=CL2[:, col:col + 1])),
                     reads=[rk, "SM"], writes=["WK6"])
                p.op(A, (lambda e: e.activation(out=TMP[:, 0:T], in_=TMP[:, 0:T], func=AF.Sqrt, bias=ONEC, scale=-1.0)),
                     reads=["WK6", "SM"], writes=["WK6"])
                p.op(A, (lambda e, R=R, col=col: e.activation(out=R[:, 0:T], in_=R[:, 0:T], func=AF.Exp,
                                                               scale=CL[:, col:col + 1])),
                     reads=[rk, "SM"], writes=[rk])
                p.op(V, (lambda e, I_=I_: e.tensor_tensor(I_[:, 0:T], I_[:, 0:T], XC[:, 0:T], ALU.mult)),
                     reads=[ik, "WK1"], writes=[ik])
                p.op(V, (lambda e, I_=I_: e.tensor_tensor(I_[:, 0:T], I_[:, 0:T], TMP[:, 0:T], ALU.mult)),
                     reads=[ik, "WK6"], writes=[ik])
                if d_ == 0:
                    p.op(V, (lambda e, R=R, I_=I_, H=H: e.tensor_tensor_scan(
                        out=H[:, 0:T], data0=R[:, 0:T], data1=I_[:, 0:T], initial=0.0, op0=ALU.mult, op1=ALU.add)),
                        reads=[rk, ik], writes=[hk])
                else:
                    p.op(V, (lambda e, R=R, I_=I_, H=H: e.tensor_tensor_scan(
                        out=H[:, 0:T][:, ::-1], data0=R[:, 0:T][:, ::-1],
                        data1=I_[:, 0:T][:, ::-1], initial=0.0, op0=ALU.mult, op1=ALU.add)),
                        reads=[rk, ik], writes=[hk])
            GL = WKf[7]
            zchunk(4 + c, GL, "WK7", 0)
            p.op(A, lambda e: e.activation(out=GL[:, 0:T], in_=GL[:, 0:T], func=AF.Gelu_apprx_tanh),
                 reads=["WK7"], writes=["WK7"])
            H0, H1 = WKf[8], WKf[9]
            p.op(G, lambda e: e.tensor_tensor(H0[:, 0:T], H0[:, 0:T], H1[:, 0:T], ALU.add), reads=["WK8", "WK9"], writes=["WK8"])
            p.op(V, lambda e: e.tensor_tensor(H0[:, 0:T], H0[:, 0:T], GL[:, 0:T], ALU.mult), reads=["WK8", "WK7"], writes=["WK8"])
            p.op(A, (lambda e, c=c: e.copy(out=YB[:, c * T:(c + 1) * T], in_=H0[:, 0:T])), reads=["WK8"], writes=["YB"])
            p.op(G, lambda e: e.tensor_tensor(H1[:, 0:T], H0[:, 0:T], H0[:, 0:T], ALU.mult), reads=["WK8"], writes=["WK9"])
            sumsq_cols(H1, "WK9", c * 16)
            cv(12)

        p.op(G, lambda e: e.memset(WKb[3][:, 0:16], 0.0), writes=["WK3"])
        p.op(G, lambda e: e.memset(WKb[3][:, 14 + T:32 + T], 0.0), writes=["WK3"])
        for c in range(4):
            Aa, Bb, GLU, DG, CV, Dd, DSQ, RS = WKf[1], WKf[2], WKb[3], WKb[4], WKf[5], WKf[6], WKf[7], WKf[8]
            zchunk(8 + c, Aa, "WK1", 0)
            zchunk(12 + c, Bb, "WK2", 0)
            p.op(A, lambda e: e.activation(out=Bb[:, 0:T], in_=Bb[:, 0:T], func=AF.Sigmoid), reads=["WK2"], writes=["WK2"])
            p.op(V, lambda e: e.tensor_tensor(GLU[:, 15:15 + T], Aa[:, 0:T], Bb[:, 0:T], ALU.mult),
                 reads=["WK1", "WK2"], writes=["WK3"])
            for k in range(31):
                p.op(V, (lambda e, k=k, c=c: e.tensor_scalar(DG[:, k * 128:(k + 1) * 128], identb[:],
                                                             PP[:, 52 + k * 4 + c: 53 + k * 4 + c], 0.0, ALU.mult, ALU.add)),
                     reads=["identb", "PP"], writes=["WK4"])
            for tt in range(4):
                b = gbank()
                for k in range(31):
                    p.op(P_, (lambda e, b=b, k=k, tt=tt: e.matmul(bank(b), DG[:, k * 128:(k + 1) * 128],
                                                                  GLU[:, tt * 512 + k: tt * 512 + k + 512],
                                                                  start=(k == 0), stop=(k == 30))),
                         reads=["WK4", "WK3"], writes=[bk(b)])
                p.op(A, (lambda e, b=b, tt=tt, c=c: e.activation(out=CV[:, tt * 512:(tt + 1) * 512], in_=bank(b),
                                                                 func=AF.Identity, bias=PP[:, 176 + c:177 + c])),
                     reads=[bk(b), "PP"], writes=["WK5"])
            for tt in range(4):
                b = gbank()
                p.op(P_, (lambda e, b=b, tt=tt: e.matmul(bank(b), GM[:], CV[:, tt * 512:(tt + 1) * 512], start=True, stop=True)),
                     reads=["GM", "WK5"], writes=[bk(b)])
                p.op(V, (lambda e, b=b, tt=tt: e.tensor_tensor(Dd[:, tt * 512:(tt + 1) * 512], CV[:, tt * 512:(tt + 1) * 512],
                                                               bank(b), ALU.subtract)),
                     reads=[bk(b), "WK5"], writes=["WK6"])
            p.op(G, lambda e: e.tensor_tensor(DSQ[:, 0:T], Dd[:, 0:T], Dd[:, 0:T], ALU.mult), reads=["WK6"], writes=["WK7"])
            for tt in range(4):
                b = gbank()
                p.op(P_, (lambda e, b=b, tt=tt: e.matmul(bank(b), GM[:], DSQ[:, tt * 512:(tt + 1) * 512], start=True, stop=True)),
                     reads=["GM", "WK7"], writes=[bk(b)])
                p.op(A, (lambda e, b=b, tt=tt: e.activation(out=RS[:, tt * 512:(tt + 1) * 512], in_=bank(b), func=AF.Sqrt,
                                                            bias=EPSC, scale=1.0)),
                     reads=[bk(b), "SM"], writes=["WK8"])
            p.op(V, lambda e: e.reciprocal(RS[:, 0:T], RS[:, 0:T]), reads=["WK8"], writes=["WK8"])
            p.op(V, lambda e: e.tensor_tensor(Dd[:, 0:T], Dd[:, 0:T], RS[:, 0:T], ALU.mult), reads=["WK6", "WK8"], writes=["WK6"])
            p.op(A, (lambda e, c=c: e.activation(out=Dd[:, 0:T], in_=Dd[:, 0:T], func=AF.Silu,
                                                 bias=PP[:, 184 + c:185 + c], scale=PP[:, 180 + c:181 + c])),
                 reads=["WK6", "PP"], writes=["WK6"])
            p.op(A, (lambda e, c=c: e.copy(out=YB[:, (4 + c) * T:(5 + c) * T], in_=Dd[:, 0:T])), reads=["WK6"], writes=["YB"])
            p.op(G, lambda e: e.tensor_tensor(DSQ[:, 0:T], Dd[:, 0:T], Dd[:, 0:T], ALU.mult), reads=["WK6"], writes=["WK7"])
            sumsq_cols(DSQ, "WK7", 64 + c * 16)
            cv(12)

        p.barrier()
        p.op(V, lambda e: e.tensor_reduce(out=sub(RS2, 0, [[16, 2], [1, 16]]), in_=sub(bank(0), 0, [[64, 2], [1, 16], [16, 4]]), axis=AX.X, op=ALU.add),
             reads=[bk(0)], writes=["SMr"])
        p.op(A, lambda e: e.activation(out=RS2, in_=RS2, func=AF.Sqrt, bias=EPSC, scale=1.0 / 512), reads=["SMr", "SM"], writes=["SMr"])
        p.op(V, lambda e: e.reciprocal(RS2, RS2), reads=["SMr"], writes=["SMr"])
        for cc in range(8):
            stg = WK[2][:, (cc % 2) * 1024:(cc % 2) * 1024 + 1024]
            skey = "WK2_%d" % (cc % 2)
            wob = WKb[cc // 4][:, (cc % 4) * 1024:(cc % 4) * 1024 + 1024]
            p.dma("sync", (lambda e, cc=cc, stg=stg: e.dma_start(out=stg, in_=wout_d[cc * 128:(cc + 1) * 128, :])),
                  writes=[skey])
            p.op(V, (lambda e, cc=cc, stg=stg, wob=wob: e.tensor_scalar(wob, stg, PP[:, 188 + cc:189 + cc], 0.0, ALU.mult, ALU.add)),
                 reads=[skey, "PP"], writes=["WOB"])

        def h2tile(ti):
            if ti < 8:
                return XTBf[:, ti * 1024:(ti + 1) * 1024]
            return WINBf[:, (ti - 8) * 1024:(ti - 7) * 1024]

        for ti in range(16):
            XS = WK[3][:, (ti % 2) * 1024:(ti % 2) * 1024 + 1024]
            xkey = "XS%d" % (ti % 2)
            p.dma("sync", (lambda e, ti=ti, XS=XS: e.dma_start(out=XS, in_=x_d[ti * 128:(ti + 1) * 128, :])),
                  writes=[xkey])
            cv(1)
            H2t = h2tile(ti)
            hkey = "XTB" if ti < 8 else "WINB"
            for dh in range(2):
                bl, bc = 4 + dh, 6 + dh
                for c in range(4):
                    p.op(P_, (lambda e, bl=bl, c=c, ti=ti, dh=dh: e.matmul(
                        bank(bl), YB[:, c * T + ti * 128: c * T + ti * 128 + 128],
                        WKb[c // 4][:, (c % 4) * 1024 + dh * 512:(c % 4) * 1024 + dh * 512 + 512],
                        start=(c == 0), stop=(c == 3))), reads=["YB", "WOB"], writes=[bk(bl)])
                for c in range(4, 8):
                    p.op(P_, (lambda e, bc=bc, c=c, ti=ti, dh=dh: e.matmul(
                        bank(bc), YB[:, c * T + ti * 128: c * T + ti * 128 + 128],
                        WKb[c // 4][:, (c % 4) * 1024 + dh * 512:(c % 4) * 1024 + dh * 512 + 512],
                        start=(c == 4), stop=(c == 7))), reads=["YB", "WOB"], writes=[bk(bc)])
                p.op(V, (lambda e, bl=bl, ti=ti, dh=dh, XS=XS, H2t=H2t: e.scalar_tensor_tensor(
                    out=H2t[:, dh * 512:(dh + 1) * 512], in0=bank(bl), scalar=RS2[:, ti:ti + 1],
                    in1=XS[:, dh * 512:(dh + 1) * 512], op0=ALU.mult, op1=ALU.add)),
                    reads=[bk(bl), "SMr", xkey], writes=[hkey])
                p.op(V, (lambda e, bc=bc, ti=ti, dh=dh, H2t=H2t: e.scalar_tensor_tensor(
                    out=H2t[:, dh * 512:(dh + 1) * 512], in0=bank(bc), scalar=RS2[:, 16 + ti:17 + ti],
                    in1=H2t[:, dh * 512:(dh + 1) * 512], op0=ALU.mult, op1=ALU.add)),
                    reads=[bk(bc), "SMr", hkey], writes=[hkey])

        p.barrier()

        if stop_after == "A":
            for ti in range(16):
                p.dma("sync", (lambda e, ti=ti: e.dma_start(out=out_d[ti * 128:(ti + 1) * 128, :], in_=h2tile(ti))),
                      writes=["out%d" % ti])
            p.finish("sync", ["out%d" % ti for ti in range(16)])
            p.emit()
            return nc

        IDXT = YBu[:, 0:2048]
        SKT = YBf[:, 2048:4096]
        G2B = YBf[:, 4096:5120]
        GATET = YBf[:, 6144:8192]
        p.dma("sync", lambda e: e.dma_start(out=G2B, in_=g2_d.partition_broadcast(128)), writes=["G2B"])
        p.dma("sync", lambda e: e.dma_start(out=sub(SKT, 0, [[128, 16], [1, 128]]), in_=skT_d.rearrange("e c k -> c e k")),
              writes=["SKT"])
        WQB = [WKb[4 + dc // 2][:, (dc % 2) * 2048:(dc % 2) * 2048 + 2048] for dc in range(8)]
        for dc in range(8):
            stg = WK[8 + dc % 2][:, 0:2048]
            skey = "WQS%d" % (dc % 2)
            p.dma("sync", (lambda e, dc=dc, stg=stg: e.dma_start(out=stg, in_=wq_d[dc * 128:(dc + 1) * 128, :])), writes=[skey])
            p.op(V, (lambda e, dc=dc, stg=stg: e.tensor_copy(WQB[dc], stg)), reads=[skey], writes=["WQB"])
        for ti in range(16):
            p.op(A, (lambda e, ti=ti: e.activation(out=WK[1][:, 1024:2048], in_=h2tile(ti), func=AF.Square,
                                                   accum_out=SS2[:, ti:ti + 1])),
                 reads=[("h2", ti)], writes=["junkA", ("ss2", ti)])
        p.op(A, lambda e: e.activation(out=RSB, in_=SS2, func=AF.Sqrt, bias=EPSC, scale=1.0 / D),
             reads=[("ss2", ti) for ti in range(16)] + ["SM"], writes=["RSB"])
        p.op(V, lambda e: e.reciprocal(RSB, RSB), reads=["RSB"], writes=["RSB"])

        def reg(i, n=128):
            return WK[2][:, i * 128:i * 128 + n]

        TOP, TOPI, TOPIf = reg(0, 256), reg(2, 256).bitcast(U32), reg(4, 256)
        WRK, WRK2 = reg(6), reg(7, 256)
        TS, POS = reg(9), reg(10).bitcast(U32)
        PAu, PBu = reg(11).bitcast(U32), reg(12).bitcast(U32)
        PAf, PBf = reg(13), reg(14)
        E1, E2 = reg(15), WK[1][:, 0:128]
        EIDX, EX, GATE = WK[1][:, 128:256], WK[1][:, 256:384], WK[1][:, 384:512]
        Zs, RZ = WK[1][:, 512:520], WK[1][:, 520:528]
        C_ = WKf[0]
        OHt = RSTDb[:, 2048:4096]

        def n2tile(slot):
            return RSTDb[:, slot * 1024:(slot + 1) * 1024]

        def compute_n2(ti, slot):
            p.op(V, (lambda e, ti=ti, slot=slot: e.scalar_tensor_tensor(
                out=n2tile(slot), in0=h2tile(ti), scalar=RSB[:, ti:ti + 1], in1=G2B, op0=ALU.mult, op1=ALU.mult)),
                reads=[("h2", ti), "RSB", "G2B"], writes=[("n2", slot)])

        SBUFS = [[WK[8][:, 0:1024], WK[8][:, 1024:2048]], [WK[1][:, 1024:2048], YBf[:, 5120:6144]]]
        def b1_stage1(ti):
            cv(8)
            if lvl < 1:
                return
            slot = ti % 2
            compute_n2(ti, slot)
            N2 = n2tile(slot)
            PT = PS2[0][:, 0:512].bitcast(BF16)
            for dc in range(8):
                p.op(P_, (lambda e, dc=dc, N2=N2, PT=PT: e.transpose(PT[:, dc * 128:(dc + 1) * 128], N2[:, dc * 128:(dc + 1) * 128], identb[:])),
                     reads=[("n2", slot), "identb"], writes=[bk(0)])
            N2T = WKb[3][:, slot * 1024:(slot + 1) * 1024]
            p.op(A, (lambda e, N2T=N2T, PT=PT: e.copy(out=N2T, in_=PT)), reads=[bk(0)], writes=[("n2t", slot)])
            if lvl < 2:
                return
            QSB = WKf[9]
            Sh = SBUFS[ti % 2]
            for half in range(2):
                QP = PS2[1]
                for e8 in range(8):
                    ec = half * 8 + e8
                    for dc in range(8):
                        p.op(P_, (lambda e, e8=e8, ec=ec, dc=dc, N2T=N2T, QP=QP: e.matmul(
                            QP[:, e8 * 128:(e8 + 1) * 128], WQB[dc][:, ec * 128:(ec + 1) * 128],
                            N2T[:, dc * 128:(dc + 1) * 128], start=(dc == 0), stop=(dc == 7))),
                            reads=["WQB", ("n2t", slot)], writes=["QP"])
                p.op(A, (lambda e, half=half, QP=QP: e.copy(out=QSB[:, half * 1024:(half + 1) * 1024], in_=QP[:, :])),
                     reads=["QP"], writes=[("qsb", half)])
                SP = PS2[2 + half]
                for e8 in range(8):
                    ec = half * 8 + e8
                    p.op(P_, (lambda e, e8=e8, ec=ec, SP=SP: e.matmul(
                        SP[:, e8 * 128:(e8 + 1) * 128], QSB[:, ec * 128:(ec + 1) * 128], SKT[:, ec * 128:(ec + 1) * 128],
                        start=True, stop=True)), reads=[("qsb", half), "SKT"], writes=[("SP", half)])
                p.op(A, (lambda e, half=half, SP=SP, Sh=Sh: e.copy(out=Sh[half], in_=SP[:, :])),
                     reads=[("SP", half)], writes=[("S", ti % 2, half)])
        def b1_stage2(ti):
            Sh = SBUFS[ti % 2]
            if lvl < 3:
                return
            for g in range(16):
                Sg = Sh[g // 8][:, (g % 8) * 128:(g % 8) * 128 + 128]
                sk = ("S", ti % 2, g // 8)
                t0, t1 = TOP[:, g * 16:g * 16 + 8], TOP[:, g * 16 + 8:g * 16 + 16]
                i0, i1 = TOPI[:, g * 16:g * 16 + 8], TOPI[:, g * 16 + 8:g * 16 + 16]
                p.op(V, (lambda e, Sg=Sg, t0=t0: e.max(out=t0, in_=Sg)), reads=[sk], writes=["TOP"])
                p.op(V, (lambda e, Sg=Sg, t0=t0, i0=i0: e.max_index(out=i0, in_max=t0, in_values=Sg)), reads=[sk, "TOP"], writes=["TOPI"])
                p.op(V, (lambda e, Sg=Sg, t0=t0: e.match_replace(out=WRK, in_to_replace=t0, in_values=Sg, imm_value=-1e30)),
                     reads=[sk, "TOP"], writes=["WRK"])
                p.op(V, (lambda e, t1=t1: e.max(out=t1, in_=WRK)), reads=["WRK"], writes=["TOP"])
                p.op(V, (lambda e, t1=t1, i1=i1: e.max_index(out=i1, in_max=t1, in_values=WRK)), reads=["WRK", "TOP"], writes=["TOPI"])
            p.op(V, lambda e: e.tensor_copy(TOPIf, TOPI), reads=["TOPI"], writes=["TOPIf"])
            if lvl < 4:
                return
            p.op(V, lambda e: e.tensor_tensor(sub(C_, 0, [[256, 8], [16, 16], [1, 16]]),
                                              sub(TOP, 0, [[32, 8], [1, 16], [0, 16]]),
                                              sub(TOP, 16, [[32, 8], [0, 16], [1, 16]]), ALU.add),
                 reads=["TOP"], writes=["C"])
            for h in range(8):
                Ch = C_[:, h * 256:(h + 1) * 256]
                t0, t1 = TS[:, h * 16:h * 16 + 8], TS[:, h * 16 + 8:h * 16 + 16]
                i0, i1 = POS[:, h * 16:h * 16 + 8], POS[:, h * 16 + 8:h * 16 + 16]
                p.op(V, (lambda e, Ch=Ch, t0=t0: e.max(out=t0, in_=Ch)), reads=["C"], writes=["TS"])
                p.op(V, (lambda e, Ch=Ch, t0=t0, i0=i0: e.max_index(out=i0, in_max=t0, in_values=Ch)), reads=["C", "TS"], writes=["POS"])
                p.op(V, (lambda e, Ch=Ch, t0=t0: e.match_replace(out=WRK2, in_to_replace=t0, in_values=Ch, imm_value=-1e30)),
                     reads=["C", "TS"], writes=["WRK2"])
                p.op(V, (lambda e, t1=t1: e.max(out=t1, in_=WRK2)), reads=["WRK2"], writes=["TS"])
                p.op(V, (lambda e, t1=t1, i1=i1: e.max_index(out=i1, in_max=t1, in_values=WRK2)), reads=["WRK2", "TS"], writes=["POS"])
            if lvl < 5:
                return
            p.op(V, lambda e: e.tensor_single_scalar(PAu, POS, 4, ALU.logical_shift_right), reads=["POS"], writes=["PAu"])
            p.op(V, lambda e: e.tensor_single_scalar(PBu, POS, 15, ALU.bitwise_and), reads=["POS"], writes=["PBu"])
            p.op(V, lambda e: e.tensor_copy(PAf, PAu), reads=["PAu"], writes=["PAf"])
            p.op(V, lambda e: e.tensor_copy(PBf, PBu), reads=["PBu"], writes=["PBf"])
            for (PXf, pk, off, Eo, ek) in ((PAf, "PAf", 0, E1, "E1"), (PBf, "PBf", 16, E2, "E2")):
                p.op(V, (lambda e, PXf=PXf: e.tensor_tensor(sub(OHt, 0, [[256, 8], [16, 16], [1, 16]]),
                                                            sub(IOTA, 0, [[0, 8], [0, 16], [1, 16]]),
                                                            sub(PXf, 0, [[16, 8], [1, 16], [0, 16]]), ALU.is_equal)),
                     reads=[pk, "SM"], writes=["OH"])
                p.op(V, (lambda e, off=off: e.tensor_tensor(sub(OHt, 0, [[256, 8], [16, 16], [1, 16]]),
                                                            sub(OHt, 0, [[256, 8], [16, 16], [1, 16]]),
                                                            sub(TOPIf, off, [[32, 8], [0, 16], [1, 16]]), ALU.mult)),
                     reads=["OH", "TOPIf"], writes=["OH"])
                p.op(V, (lambda e, Eo=Eo: e.tensor_reduce(out=Eo, in_=sub(OHt, 0, [[16, 128], [1, 16]]), axis=AX.X, op=ALU.add)),
                     reads=["OH"], writes=[ek])
            p.op(V, lambda e: e.scalar_tensor_tensor(out=EIDX, in0=E1, scalar=128.0, in1=E2, op0=ALU.mult, op1=ALU.add),
                 reads=["E1", "E2"], writes=["EIDX"])
            if lvl < 6:
                return
            p.op(V, lambda e: e.tensor_tensor(sub(EX, 0, [[16, 8], [1, 16]]), sub(TS, 0, [[16, 8], [1, 16]]),
                                              sub(TS, 0, [[16, 8], [0, 16]]), ALU.subtract), reads=["TS"], writes=["EX"])
            p.op(A, lambda e: e.activation(out=EX, in_=EX, func=AF.Exp), reads=["EX"], writes=["EX"])
            p.op(V, lambda e: e.tensor_reduce(out=Zs, in_=sub(EX, 0, [[16, 8], [1, 16]]), axis=AX.X, op=ALU.add), reads=["EX"], writes=["Zs"])
            p.op(V, lambda e: e.reciprocal(RZ, Zs), reads=["Zs"], writes=["RZ"])
            p.op(V, lambda e: e.tensor_tensor(sub(GATE, 0, [[16, 8], [1, 16]]), sub(EX, 0, [[16, 8], [1, 16]]),
                                              sub(RZ, 0, [[1, 8], [0, 16]]), ALU.mult), reads=["EX", "RZ"], writes=["GATE"])
            if lvl < 7:
                return
            p.op(P_, lambda e: e.matmul(PS2[0][:, 512:640], EIDX, ident[:], start=True, stop=True), reads=["EIDX", "ident"], writes=[bk(1)])
            p.op(P_, lambda e: e.matmul(PS2[0][:, 640:768], GATE, ident[:], start=True, stop=True), reads=["GATE", "ident"], writes=[bk(1)])
            p.op(V, (lambda e, ti=ti: e.tensor_copy(IDXT[:, ti * 128:(ti + 1) * 128], PS2[0][:, 512:640])), reads=[bk(1)], writes=[("idxt", ti)])
            p.op(A, (lambda e, ti=ti: e.copy(out=GATET[:, ti * 128:(ti + 1) * 128], in_=PS2[0][:, 640:768])), reads=[bk(1)], writes=[("gatet", ti)])

        b1_stage1(0)
        for ti in range(16):
            if ti + 1 < 16:
                b1_stage1(ti + 1)
            b1_stage2(ti)
        cv(NPIECE)
        p.barrier()
        if stop_after == "B1":
            for q_, (c0) in enumerate((0, 1024, 6144, 7168)):
                p.dma("sync", (lambda e, q_=q_, c0=c0: e.dma_start(out=out_d[q_ * 128:(q_ + 1) * 128, :], in_=YBf[:, c0:c0 + 1024])),
                      writes=["out%d" % q_])
            p.finish("sync", ["out%d" % q_ for q_ in range(4)])
            p.emit()
            return nc

        GFB = WK[9][:, 1024:2048]
        OUTT = WK[9][:, 0:1024]
        ZC = WKb[8][:, 0:256]
        ACTM = WK[8][:, 256:384]
        GA = WK[8][:, 384:512]
        LTG = [WKb[8][:, 1024:2048], WKb[8][:, 2048:3072]]
        JUNK = WKb[8][:, 3072:4096]
        JUNK2 = WKb[8][:, 3072:4096]
        p.dma("sync", lambda e: e.dma_start(out=GFB, in_=gf_d.partition_broadcast(128)), writes=["GFB"])
        p.op(V, lambda e: e.memset(ZC, 0.0), writes=["ZC"])
        p.op(V, lambda e: e.memset(ZC[:, 128:129], 1.0), reads=["ZC"], writes=["ZC"])
        NS = 16
        GS = 8
        SL = [WKb[s_ // 2][:, (s_ % 2) * 2048:(s_ % 2) * 2048 + 2048] for s_ in range(16)]
        CVIb = CVI[:].bitcast(BF16)
        SL += [CVIb[:, 0:2048], CVIb[:, 2048:4096], CVO[:, 0:2048], RSTDb[:, 2048:4096], YB[:, 10240:12288]]
        NS = len(SL)
        LAG = 12
        NTOK = 16 * 128
        for step in range(NTOK + LAG):
            nu, nv = step, step - LAG
            if nu < NTOK:
                ti, t, s_ = nu // 128, nu % 128, nu % NS
                if t == 0:
                    compute_n2(ti, ti % 2)
                N2 = n2tile(ti % 2)
                XBP = PS2[nu % 3]
                xk = ("xbp", nu % 3)
                p.dma(G, (lambda e, s_=s_, nu=nu: e.indirect_dma_start(
                    out=SL[s_], out_offset=None, in_=puv_d,
                    in_offset=bass.IndirectOffsetOnAxis(ap=IDXT[:, nu:nu + 1], axis=0))),
                    reads=[("idxt", ti)], writes=[("sl", s_)], stream=("g", s_))
                for half in range(2):
                    p.op(P_, (lambda e, t=t, half=half, XBP=XBP, N2=N2: e.matmul(
                        XBP[:, half * 512:(half + 1) * 512], identb[:, t:t + 1].to_broadcast([128, 128]),
                        N2[:, half * 512:(half + 1) * 512], start=True, stop=True)),
                        reads=["identb", ("n2", ti % 2)], writes=[xk])
            mm = step - 3
            if 0 <= mm < NTOK and mm % GS == GS - 1:
                ti_m, g0 = mm // 128, (mm % 128) - (GS - 1)
                gk_m = ("ga", (mm // GS) % 16)
                p.op(V, (lambda e, g0=g0, ti_m=ti_m: e.tensor_tensor(GA[:, g0:g0 + GS], GA[:, g0:g0 + GS],
                                                                     GATET[:, ti_m * 128 + g0: ti_m * 128 + g0 + GS], ALU.mult)),
                     reads=[gk_m, ("gatet", ti_m)], writes=[gk_m])
                gsl = (mm // GS) % 2
                p.op(V, (lambda e, g0=g0, gsl=gsl: e.tensor_tensor(
                    sub(LTG[gsl], 0, [[128, GS], [1, 128]]),
                    sub(ZC, 128 - g0, [[-1, GS], [1, 128]]),
                    sub(GA, g0, [[1, GS], [0, 128]]), ALU.mult)),
                    reads=["ZC", gk_m], writes=[("ltg", gsl)])
            if nv >= 0:
                ti, t, s_ = nv // 128, nv % 128, nv % NS
                OP = PS2[3]
                ok = ("op", 0)
                gsl_v = (nv // GS) % 2
                lt = LTG[gsl_v][:, (nv % GS) * 128:(nv % GS) * 128 + 128]
                for half in range(2):
                    p.op(P_, (lambda e, s_=s_, t=t, half=half, lt=lt, OP=OP: e.matmul(
                        OP[:, half * 512:(half + 1) * 512], lt, SL[s_][:, 1024 + half * 512:1024 + (half + 1) * 512],
                        start=(t == 0), stop=(t == 127))),
                        reads=[("ltg", gsl_v), ("sl", s_)], writes=[ok])
            if nu < NTOK:
                ti, t, s_ = nu // 128, nu % 128, nu % NS
                p.op(V, (lambda e, s_=s_, t=t, XBP=XBP: e.scalar_tensor_tensor(
                    out=JUNK, in0=SL[s_][:, 0:1024], scalar=1.0, in1=XBP[:, :], op0=ALU.mult, op1=ALU.mult,
                    accum_out=ACTM[:, t:t + 1])),
                    reads=[("sl", s_), xk], writes=[("actm", t)])
                if t % GS == GS - 1:
                    g0 = t - (GS - 1)
                    gk = ("ga", (nu // GS) % 16)
                    p.op(A, (lambda e, g0=g0: e.activation(out=GA[:, g0:g0 + GS], in_=ACTM[:, g0:g0 + GS], func=AF.Gelu_apprx_tanh)),
                         reads=[("actm", tt_) for tt_ in range(g0, g0 + GS)], writes=[gk])
            if nv >= 0 and nv % 128 == 127:
                ti = nv // 128
                OP = PS2[3]
                ok = ("op", 0)
                p.op(V, (lambda e, ti=ti, OP=OP: e.tensor_tensor(OUTT, h2tile(ti), OP[:, :], ALU.add)),
                     reads=[("h2", ti), ok], writes=["OUTT"])
                p.op(A, lambda e: e.activation(out=JUNK2, in_=OUTT, func=AF.Square, accum_out=SS3), reads=["OUTT"], writes=["JUNK2", "SS3"])
                p.op(A, lambda e: e.activation(out=R3, in_=SS3, func=AF.Sqrt, bias=EPSC, scale=1.0 / D), reads=["SS3", "SM"], writes=["R3"])
                p.op(V, lambda e: e.reciprocal(R3, R3), reads=["R3"], writes=["R3"])
                p.op(V, lambda e: e.scalar_tensor_tensor(out=OUTT, in0=OUTT, scalar=R3, in1=GFB, op0=ALU.mult, op1=ALU.mult),
                     reads=["OUTT", "R3", "GFB"], writes=["OUTT"])
                p.dma("sync", (lambda e, ti=ti: e.dma_start(out=out_d[ti * 128:(ti + 1) * 128, :], in_=OUTT)),
                      reads=["OUTT"], writes=["out%d" % ti])
        p.finish("sync", ["out%d" % ti for ti in range(16)])
        p.emit()
    return nc


def make_in_maps(x, mix_norm_g, w_in, lru_conv_w, lru_conv_b, lru_w_rg, lru_b_rg, lru_w_ig, lru_b_ig,
                 lru_lambda, conf_conv_w, conf_conv_b, conf_norm_g, conf_norm_b, beta_lru, beta_conv,
                 w_out, ffn_norm_g, peer_w_q, peer_sub_keys, peer_u, peer_v, final_norm_g):
    f = lambda a: np.ascontiguousarray(np.asarray(a, dtype=np.float32))

    def cols(v):
        return f(v).reshape(-1, 128).T

    pp = np.concatenate([
        cols(mix_norm_g[0]),
        np.concatenate([cols(lru_conv_w[0][k]) for k in range(4)], axis=1),
        cols(lru_conv_b[0]),
        cols(lru_b_rg[0][0]), cols(lru_b_ig[0][0]), cols(lru_b_rg[0][1]), cols(lru_b_ig[0][1]),
        cols(lru_lambda[0][0]), cols(lru_lambda[0][1]),
        np.concatenate([cols(conf_conv_w[0][k]) for k in range(31)], axis=1),
        cols(conf_conv_b[0]), cols(conf_norm_g[0]), cols(conf_norm_b[0]),
        cols(beta_lru[0]), cols(beta_conv[0]),
    ], axis=1)
    pp = f(pp)
    assert pp.shape == (128, NPP)
    wg = f(np.stack([lru_w_rg[0][0], lru_w_ig[0][0], lru_w_rg[0][1], lru_w_ig[0][1]], axis=0))
    skT = f(np.transpose(np.asarray(peer_sub_keys[0]), (0, 1, 3, 2)).reshape(16, 128, 128))
    shared = {
        "w_in": f(w_in[0]), "w_out": f(w_out[0]), "w_q": f(peer_w_q[0]), "skT": skT,
        "pu": f(peer_u[0]), "pv": f(peer_v[0]), "pp": pp, "wg": wg,
        "g2": f(ffn_norm_g[0]).reshape(1, D), "gf": f(final_norm_g).reshape(1, D),
    }
    xs = np.asarray(x, dtype=np.float32)
    maps = []
    for b in range(xs.shape[0]):
        m = dict(shared)
        m["x"] = f(xs[b])
        m["xT"] = f(xs[b].T)
        maps.append(m)
    return maps


_NC = None


def kernel(**inputs):
    global _NC
    in_maps = make_in_maps(**inputs)
    if _NC is None:
        _NC = build()
    res = run_bass_kernel_spmd(_NC, in_maps, core_ids=list(range(len(in_maps))))
    return np.stack([np.asarray(r["out"], dtype=np.float32) for r in res.results], axis=0)
```
